# Optimizing a Trainium2 kernel written in Bass

```python
import math
import jax, jax.numpy as jnp
from jax import lax
import numpy as np

D_MODEL = 1024
BATCH = 2
SEQ = 8192
DEPTH = 1

D_MIX = D_MODEL
D_ATTN = D_MIX // 2
D_RNN = D_MIX - D_ATTN
HEAD_DIM = 64
N_HEADS = D_ATTN // HEAD_DIM
N_KV_HEADS = 2
GQA_REP = N_HEADS // N_KV_HEADS
D_KV = N_KV_HEADS * HEAD_DIM
CMP_LEN = 32
CMP_STRIDE = 16
SEL_BLOCK = 64
N_SEL = 16
WINDOW = 512
Q_BLOCK = 128
FORCE_BONUS = 1e4
N_BUCKETS = 32
MAX_DISTANCE = 128
RG_BLOCKS = 8
RG_BLOCK_DIM = D_RNN // RG_BLOCKS
CONV_WIDTH = 4
RG_C = 8.0
N_GROUPS = 4
EXPERTS_PER_GROUP = 4
N_EXPERTS = N_GROUPS * EXPERTS_PER_GROUP
TOP_K_INNER = 2
D_FF_EXPERT = 512
D_PLE = 256
ALPHA = (2.0 * DEPTH) ** 0.25
BETA = (8.0 * DEPTH) ** -0.25
LN_EPS = 1e-5
RMS_EPS = 1e-6
NEG = -1e30
N_GATE_COLS = 3 * N_HEADS
SPLIT_IDX = [D_ATTN, D_ATTN + 6 * D_KV, D_ATTN + 6 * D_KV + N_GATE_COLS,
             D_ATTN + 6 * D_KV + N_GATE_COLS + D_RNN]
D_IN_PROJ = D_ATTN + 6 * D_KV + N_GATE_COLS + 2 * D_RNN

kernel_name = 'hymba_nsa_rglru_hmoe_deepnorm'


def layer_norm(x, g, b):
    xf = x.astype(jnp.float32)
    mu = jnp.mean(xf, axis=-1, keepdims=True)
    var = jnp.mean(jnp.square(xf - mu), axis=-1, keepdims=True)
    y = (xf - mu) * lax.rsqrt(var + LN_EPS) * g.astype(jnp.float32) + b.astype(jnp.float32)
    return y.astype(x.dtype)


def rms_norm(x, g):
    xf = x.astype(jnp.float32)
    y = xf * lax.rsqrt(jnp.mean(jnp.square(xf), axis=-1, keepdims=True) + RMS_EPS)
    return y * g.astype(jnp.float32)


def t5_bucket(dist):
    max_exact = N_BUCKETS // 2
    d = jnp.maximum(dist, 0)
    df = jnp.maximum(d, 1).astype(jnp.float32)
    large = max_exact + (jnp.log(df / max_exact) / math.log(MAX_DISTANCE / max_exact)
                         * (N_BUCKETS - max_exact)).astype(jnp.int32)
    large = jnp.minimum(large, N_BUCKETS - 1)
    return jnp.where(d < max_exact, d, large)


def masked_softmax(logits, mask):
    lf = jnp.where(mask, logits.astype(jnp.float32), NEG)
    m = jnp.max(lf, axis=-1, keepdims=True)
    e = jnp.where(mask, jnp.exp(lf - m), 0.0)
    return e / jnp.maximum(jnp.sum(e, axis=-1, keepdims=True), 1e-30)


def nsa_attention(q, k_cmp_raw, v_cmp_raw, k_slc, v_slc, k_win, v_win, gates,
                  cmp_pe_k, cmp_pe_v, cmp_w_k, cmp_w_v, rel_bias):
    B, T = q.shape[0], q.shape[1]
    G, R, Dh = N_KV_HEADS, GQA_REP, HEAD_DIM
    n_cmp = (T - CMP_LEN) // CMP_STRIDE + 1
    n_slc = T // SEL_BLOCK
    n_sel = min(N_SEL, n_slc)
    n_qblk = T // Q_BLOCK
    scale = Dh ** -0.5

    tok = jnp.arange(n_cmp)[:, None] * CMP_STRIDE + jnp.arange(CMP_LEN)[None, :]

    def compress(raw, pe, w):
        blk = raw[:, tok] + pe[None, None, :, None, :]
        blk = jnp.moveaxis(blk, 3, 2).reshape(B, n_cmp, G, CMP_LEN * Dh)
        return blk @ w

    k_c = compress(k_cmp_raw, cmp_pe_k, cmp_w_k)
    v_c = compress(v_cmp_raw, cmp_pe_v, cmp_w_v)
    cmp_end = jnp.arange(n_cmp) * CMP_STRIDE + CMP_LEN - 1
    cs = jnp.arange(n_cmp) * CMP_STRIDE
    ss = jnp.arange(n_slc) * SEL_BLOCK
    overlap = ((cs[:, None] < ss[None, :] + SEL_BLOCK) &
               (cs[:, None] + CMP_LEN > ss[None, :])).astype(jnp.float32)

    k_s_t = jnp.moveaxis(k_slc, 1, 2)
    v_s_t = jnp.moveaxis(v_slc, 1, 2)
    k_w_p = jnp.pad(k_win, ((0, 0), (WINDOW, 0), (0, 0), (0, 0)))
    v_w_p = jnp.pad(v_win, ((0, 0), (WINDOW, 0), (0, 0), (0, 0)))

    tab = rel_bias.T.reshape(G, R, N_BUCKETS)
    g_ix = jnp.arange(G)[None, :, None, None, None]
    r_ix = jnp.arange(R)[None, None, :, None, None]
    sblk = jnp.arange(n_slc)

    def block(i):
        qs = i * Q_BLOCK
        qpos = qs + jnp.arange(Q_BLOCK)
        qb = lax.dynamic_slice_in_dim(q, qs, Q_BLOCK, axis=1).reshape(B, Q_BLOCK, G, R, Dh) * scale
        gb = lax.dynamic_slice_in_dim(gates, qs, Q_BLOCK, axis=1)

        dist_c = qpos[:, None] - cmp_end[None, :]
        s_c = jnp.einsum('bqgrd,bcgd->bgrqc', qb, k_c) + tab[:, :, t5_bucket(dist_c)]
        p_c = masked_softmax(s_c, dist_c >= 0)
        o_c = jnp.einsum('bgrqc,bcgd->bqgrd', p_c, v_c)

        imp = jnp.einsum('bgrqc,cs->bgqs', p_c, overlap)
        cur = (qpos // SEL_BLOCK)[:, None]
        valid = sblk[None, :] <= cur
        forced = (sblk[None, :] == 0) | (sblk[None, :] == cur) | (sblk[None, :] == cur - 1)
        score = jnp.where(valid, imp + jnp.where(forced, FORCE_BONUS, 0.0), NEG)
        _, sel = lax.top_k(score, n_sel)

        kidx = (sel[..., None] * SEL_BLOCK + jnp.arange(SEL_BLOCK)).reshape(B, G, Q_BLOCK * n_sel * SEL_BLOCK)
        ks = jnp.take_along_axis(k_s_t, kidx[..., None], axis=2).reshape(B, G, Q_BLOCK, n_sel * SEL_BLOCK, Dh)
        vs = jnp.take_along_axis(v_s_t, kidx[..., None], axis=2).reshape(B, G, Q_BLOCK, n_sel * SEL_BLOCK, Dh)
        kpos = kidx.reshape(B, G, Q_BLOCK, n_sel * SEL_BLOCK)
        dist_s = qpos[None, None, :, None] - kpos
        bias_s = tab[g_ix, r_ix, t5_bucket(dist_s)[:, :, None]]
        s_s = jnp.einsum('bqgrd,bgqkd->bgrqk', qb, ks) + bias_s
        p_s = masked_softmax(s_s, (dist_s >= 0)[:, :, None])
        o_s = jnp.einsum('bgrqk,bgqkd->bqgrd', p_s, vs)

        kw = lax.dynamic_slice_in_dim(k_w_p, qs, WINDOW + Q_BLOCK, axis=1)
        vw = lax.dynamic_slice_in_dim(v_w_p, qs, WINDOW + Q_BLOCK, axis=1)
        wpos = qs - WINDOW + jnp.arange(WINDOW + Q_BLOCK)
        dist_w = qpos[:, None] - wpos[None, :]
        mask_w = (dist_w >= 0) & (dist_w < WINDOW) & (wpos[None, :] >= 0)
        s_w = jnp.einsum('bqgrd,bkgd->bgrqk', qb, kw) + tab[:, :, t5_bucket(dist_w)]
        p_w = masked_softmax(s_w, mask_w)
        o_w = jnp.einsum('bgrqk,bkgd->bqgrd', p_w, vw)

        out = gb[..., 0:1] * o_c + gb[..., 1:2] * o_s + gb[..., 2:3] * o_w
        return out.reshape(B, Q_BLOCK, N_HEADS * Dh)

    out = lax.map(block, jnp.arange(n_qblk))
    return jnp.moveaxis(out, 0, 1).reshape(B, T, N_HEADS * Dh)


def rglru_branch(xr, yr, conv_w, conv_b, w_a, b_a, w_x, b_x, lam):
    B, T, C = xr.shape
    xf = xr.astype(jnp.float32)
    xc = lax.conv_general_dilated(xf, conv_w.astype(jnp.float32).reshape(CONV_WIDTH, 1, C),
                                  window_strides=(1,), padding=[(CONV_WIDTH - 1, 0)],
                                  dimension_numbers=('NWC', 'WIO', 'NWC'),
                                  feature_group_count=C) + conv_b.astype(jnp.float32)
    xb = xc.reshape(B, T, RG_BLOCKS, RG_BLOCK_DIM)
    r = jax.nn.sigmoid(jnp.einsum('btnk,nkj->btnj', xb, w_a.astype(jnp.float32)).reshape(B, T, C) + b_a)
    i_g = jax.nn.sigmoid(jnp.einsum('btnk,nkj->btnj', xb, w_x.astype(jnp.float32)).reshape(B, T, C) + b_x)
    log_a = -RG_C * r * jax.nn.softplus(-lam.astype(jnp.float32))
    a = jnp.exp(log_a)
    bterm = jnp.sqrt(-jnp.expm1(2.0 * log_a)) * (i_g * xc)

    def combine(c1, c2):
        a1, b1 = c1
        a2, b2 = c2
        return a1 * a2, a2 * b1 + b2

    _, h = lax.associative_scan(combine, (a, bterm), axis=1)
    return h * jax.nn.gelu(yr.astype(jnp.float32))


def hmoe(h, wg, bg, we, be, w_up, w_down):
    B, T, D = h.shape
    t = h.reshape(B * T, D)
    n = t.shape[0]
    pg = jax.nn.softmax((t @ wg + bg).astype(jnp.float32), axis=-1)
    pg_top, gi = lax.top_k(pg, 1)
    le = (t @ we + be).astype(jnp.float32).reshape(n, N_GROUPS, EXPERTS_PER_GROUP)
    le_g = jnp.take_along_axis(le, gi[:, :, None], axis=1)[:, 0]
    pe = jax.nn.softmax(le_g, axis=-1)
    pe_top, ei = lax.top_k(pe, TOP_K_INNER)
    w = pg_top * pe_top / jnp.sum(pe_top, axis=-1, keepdims=True)
    eid = gi * EXPERTS_PER_GROUP + ei
    comb = jnp.sum(jax.nn.one_hot(eid, N_EXPERTS, dtype=jnp.float32) * w[..., None], axis=1)
    out = jnp.zeros((n, D), jnp.float32)
    for e in range(N_EXPERTS):
        u = t @ w_up[e]
        ua, ub = jnp.split(u, 2, axis=-1)
        out = out + comb[:, e:e + 1] * ((jax.nn.silu(ua) * ub) @ w_down[e])
    return out.reshape(B, T, D).astype(h.dtype)


def setup_inputs(seed: int = 0) -> dict:
    key = jax.random.key(seed)
    ks = jax.random.split(key, 32)
    f32 = jnp.float32
    nrm = lambda k, shape, s: jax.random.normal(k, shape, f32) * s
    col_scale = jnp.concatenate([
        jnp.ones((D_ATTN,), f32),
        jnp.ones((D_KV,), f32), jnp.full((D_KV,), BETA, f32),
        jnp.ones((D_KV,), f32), jnp.full((D_KV,), BETA, f32),
        jnp.ones((D_KV,), f32), jnp.full((D_KV,), BETA, f32),
        jnp.ones((N_GATE_COLS,), f32),
        jnp.full((D_RNN,), BETA, f32), jnp.ones((D_RNN,), f32)])
    u = jax.random.uniform(ks[10], (DEPTH, D_RNN), f32, 0.9, 0.999)
    a0 = u ** (1.0 / RG_C)
    return {
        'x': nrm(ks[0], (BATCH, SEQ, D_MODEL), 1.0),
        'p': nrm(ks[1], (DEPTH, BATCH, SEQ, D_PLE), 1.0),
        'rel_bias': nrm(ks[2], (N_BUCKETS, N_HEADS), 0.2),
        'w_in': nrm(ks[3], (DEPTH, D_MODEL, D_IN_PROJ), D_MODEL ** -0.5) * col_scale,
        'cmp_pe_k': nrm(ks[4], (DEPTH, CMP_LEN, HEAD_DIM), 0.02),
        'cmp_pe_v': nrm(ks[5], (DEPTH, CMP_LEN, HEAD_DIM), 0.02),
        'cmp_w_k': nrm(ks[6], (DEPTH, CMP_LEN * HEAD_DIM, HEAD_DIM), (CMP_LEN * HEAD_DIM) ** -0.5),
        'cmp_w_v': nrm(ks[7], (DEPTH, CMP_LEN * HEAD_DIM, HEAD_DIM), (CMP_LEN * HEAD_DIM) ** -0.5),
        'conv_w': nrm(ks[8], (DEPTH, CONV_WIDTH, D_RNN), CONV_WIDTH ** -0.5),
        'conv_b': nrm(ks[9], (DEPTH, D_RNN), 0.02),
        'rg_w_a': nrm(ks[11], (DEPTH, RG_BLOCKS, RG_BLOCK_DIM, RG_BLOCK_DIM), RG_BLOCK_DIM ** -0.5),
        'rg_b_a': nrm(ks[12], (DEPTH, D_RNN), 0.02),
        'rg_w_x': nrm(ks[13], (DEPTH, RG_BLOCKS, RG_BLOCK_DIM, RG_BLOCK_DIM), RG_BLOCK_DIM ** -0.5),
        'rg_b_x': nrm(ks[14], (DEPTH, D_RNN), 0.02),
        'rg_lambda': jnp.log(a0 / (1.0 - a0)),
        'attn_out_gain': 1.0 + nrm(ks[15], (DEPTH, D_ATTN), 0.02),
        'rnn_out_gain': 1.0 + nrm(ks[16], (DEPTH, D_RNN), 0.02),
        'w_out': nrm(ks[17], (DEPTH, D_MIX, D_MODEL), D_MIX ** -0.5 * BETA),
        'ln1_g': 1.0 + nrm(ks[18], (DEPTH, D_MODEL), 0.02),
        'ln1_b': nrm(ks[19], (DEPTH, D_MODEL), 0.02),
        'router_group_w': nrm(ks[20], (DEPTH, D_MODEL, N_GROUPS), D_MODEL ** -0.5),
        'router_group_b': nrm(ks[21], (DEPTH, N_GROUPS), 0.01),
        'router_expert_w': nrm(ks[22], (DEPTH, D_MODEL, N_EXPERTS), D_MODEL ** -0.5),
        'router_expert_b': nrm(ks[23], (DEPTH, N_EXPERTS), 0.01),
        'expert_w_up': nrm(ks[24], (DEPTH, N_EXPERTS, D_MODEL, 2 * D_FF_EXPERT), D_MODEL ** -0.5),
        'expert_w_down': nrm(ks[25], (DEPTH, N_EXPERTS, D_FF_EXPERT, D_MODEL), D_FF_EXPERT ** -0.5 * BETA),
        'ple_w': nrm(ks[26], (DEPTH, D_PLE, D_MODEL), D_PLE ** -0.5 * BETA),
        'ple_gate_w': nrm(ks[27], (DEPTH, D_MODEL, D_MODEL), D_MODEL ** -0.5),
        'ln2_g': 1.0 + nrm(ks[28], (DEPTH, D_MODEL), 0.02),
        'ln2_b': nrm(ks[29], (DEPTH, D_MODEL), 0.02),
    }


def reference(x, p, rel_bias, w_in, cmp_pe_k, cmp_pe_v, cmp_w_k, cmp_w_v, conv_w, conv_b,
              rg_w_a, rg_b_a, rg_w_x, rg_b_x, rg_lambda, attn_out_gain, rnn_out_gain, w_out,
              ln1_g, ln1_b, router_group_w, router_group_b, router_expert_w, router_expert_b,
              expert_w_up, expert_w_down, ple_w, ple_gate_w, ln2_g, ln2_b):
    B, T, _ = x.shape
    for i in range(DEPTH):
        proj = x @ w_in[i]
        q, kv, g, rx, ry = jnp.split(proj, SPLIT_IDX, axis=-1)
        q = q.reshape(B, T, N_HEADS, HEAD_DIM)
        k_c, v_c, k_s, v_s, k_w, v_w = [c.reshape(B, T, N_KV_HEADS, HEAD_DIM)
                                        for c in jnp.split(kv, 6, axis=-1)]
        gates = jax.nn.sigmoid(g).reshape(B, T, N_KV_HEADS, GQA_REP, 3)
        attn = nsa_attention(q, k_c, v_c, k_s, v_s, k_w, v_w, gates,
                             cmp_pe_k[i], cmp_pe_v[i], cmp_w_k[i], cmp_w_v[i], rel_bias)
        rnn = rglru_branch(rx, ry, conv_w[i], conv_b[i], rg_w_a[i], rg_b_a[i],
                           rg_w_x[i], rg_b_x[i], rg_lambda[i])
        heads = jnp.concatenate([rms_norm(attn, attn_out_gain[i]),
                                 rms_norm(rnn, rnn_out_gain[i])], axis=-1).astype(x.dtype)
        mix = heads @ w_out[i]
        x = layer_norm(ALPHA * x + mix, ln1_g[i], ln1_b[i])
        moe = hmoe(x, router_group_w[i], router_group_b[i], router_expert_w[i],
                   router_expert_b[i], expert_w_up[i], expert_w_down[i])
        ple = jax.nn.sigmoid(x @ ple_gate_w[i]) * (p[i] @ ple_w[i])
        x = layer_norm(ALPHA * x + moe + ple, ln2_g[i], ln2_b[i])
    return x
```

```python
import numpy as np
from contextlib import ExitStack
import concourse.bass as bass
import concourse.mybir as mybir
from concourse.bass_utils import run_bass_kernel_spmd

F32 = mybir.dt.float32
BF16 = mybir.dt.bfloat16
AF = mybir.ActivationFunctionType
ALU = mybir.AluOpType

COMPUTE = ("pe", "act", "dve", "pool")
STREAMS = ("pe", "act", "dve", "pool", "sp")
NRING = 8
ENGMAP = {"pe": "tensor", "act": "scalar", "dve": "vector", "pool": "gpsimd", "sp": "sync"}


class Op:
    __slots__ = ("eng", "fn", "dma", "deps", "signal", "pos", "ring", "ringval", "idx", "waits")

    def __init__(self, eng, fn, dma):
        self.eng = eng
        self.fn = fn
        self.dma = dma
        self.deps = []
        self.signal = False
        self.pos = None
        self.ring = None
        self.ringval = None
        self.waits = None


class Sched:
    def __init__(self, base):
        self.base = base
        self.streams = {e: [] for e in STREAMS}
        self.last_writer = {}
        self.readers = {}
        self.dma_count = {s: 0 for s in STREAMS}
        self.dma_hist = {s: [] for s in STREAMS}

    def add(self, eng, fn, reads=(), writes=(), dma=False):
        op = Op(eng, fn, dma)
        op.idx = len(self.streams[eng])
        deps = []
        for b in reads:
            w = self.last_writer.get(b)
            if w is not None:
                deps.append(w)
        for b in writes:
            w = self.last_writer.get(b)
            if w is not None:
                deps.append(w)
            deps.extend(self.readers.get(b, ()))
        if dma:
            k = self.dma_count[eng]
            self.dma_count[eng] += 1
            op.ring = k % NRING
            op.ringval = 16 * (k // NRING + 1)
            hist = self.dma_hist[eng]
            if k >= NRING:
                deps.append(hist[k - NRING])
            hist.append(op)
        best = {}
        for d in deps:
            if d is op:
                continue
            if d.dma:
                key = ("dma", d.eng, d.ring)
                cur = best.get(key)
                if cur is None or d.ringval > cur.ringval:
                    best[key] = d
            else:
                if d.eng == op.eng and not op.dma and op.eng == "pe":
                    continue
                cur = best.get(d.eng)
                if cur is None or d.idx > cur.idx:
                    best[d.eng] = d
        for d in best.values():
            if not d.dma:
                d.signal = True
            op.deps.append(d)
        self.streams[eng].append(op)
        for b in writes:
            self.last_writer[b] = op
            self.readers[b] = []
        for b in reads:
            self.readers.setdefault(b, []).append(op)
        return op

    def drain(self):
        op = Op("sp", None, False)
        op.idx = len(self.streams["sp"])
        for s in STREAMS:
            hist = self.dma_hist[s]
            for r in range(NRING):
                last = None
                for o in hist:
                    if o.ring == r:
                        last = o
                if last is not None:
                    op.deps.append(last)
        self.streams["sp"].append(op)

    def emit(self, block, sems):
        base = self.base
        for e in COMPUTE:
            c = 0
            for op in self.streams[e]:
                if op.signal:
                    c += 1
                    op.pos = base.get(e, 0) + c
        for s in STREAMS:
            seen = {}
            for op in self.streams[s]:
                op.waits = []
                for d in op.deps:
                    if d.dma:
                        key = (d.eng, d.ring)
                        val = base.get(key, 0) + d.ringval
                    else:
                        key = d.eng
                        val = d.pos
                    if seen.get(key, 0) >= val:
                        continue
                    seen[key] = val
                    op.waits.append((key, val))

        def body_for(sname):
            ops = self.streams[sname]

            def body(eng):
                for op in ops:
                    for key, val in op.waits:
                        eng.wait_ge(sems[key], val)
                    if op.fn is None:
                        continue
                    inst = op.fn(eng)
                    if op.dma:
                        inst.then_inc(sems[(op.eng, op.ring)], 16)
                    elif op.signal:
                        inst.then_inc(sems[op.eng], 1)
            return body

        for sname in STREAMS:
            if self.streams[sname]:
                getattr(block, ENGMAP[sname])(body_for(sname))
        nb = dict(base)
        for e in COMPUTE:
            nb[e] = base.get(e, 0) + sum(1 for op in self.streams[e] if op.signal)
        for s in STREAMS:
            for r in range(NRING):
                n = sum(1 for o in self.dma_hist[s] if o.ring == r)
                if n:
                    nb[(s, r)] = base.get((s, r), 0) + 16 * n
        return nb


D_MODEL = 1024
SEQ = 8192
NT = 64
NOWN = 16
ALPHA = 2.0 ** 0.25
LN_EPS = 1e-5
RMS_EPS = 1e-6
MASKV = -256.0
N_BUCKETS = 32
T_D, T_O1, T_W4, T_C, T_N0 = 0, 1, 2, 3, 4
NTAB = 8


def t5_bucket_np(d):
    d = np.maximum(d, 0)
    df = np.maximum(d, 1).astype(np.float32)
    large = 16 + (np.log(df / np.float32(16)) / np.float32(np.log(128 / 16)) * np.float32(16)).astype(np.int32)
    large = np.minimum(large, 31)
    return np.where(d < 16, d, large)


def _bucket_exact(d):
    import math
    d = np.maximum(d, 0)
    df = np.maximum(d, 1).astype(np.float32)
    v = np.log(df / np.float32(16.0)).astype(np.float32) / np.float32(math.log(128 / 16))
    large = 16 + (v * np.float32(16)).astype(np.int32)
    large = np.minimum(large, 31)
    return np.where(d < 16, d, large)


def build_program(dbg=()):
    nc = bass.Bass("TRN2", target_bir_lowering=False)

    def din(name, shape, dt=F32):
        return nc.dram_tensor(name, list(shape), dt, kind="ExternalInput").ap()

    def dout(name, shape, dt=F32):
        return nc.dram_tensor(name, list(shape), dt, kind="ExternalOutput").ap()

    _shapes = dict(
        xs=[SEQ, D_MODEL], po=[NOWN * 128, 256], keybias=[128, NT], cbias=[128, 4], selbias=[NOWN, 128, 128],
        padmask=[128, 512], wA=[D_MODEL, 1280], wC=[D_MODEL, 1048], tabs=[NTAB, 128, 1024], cb=[128, 8],
        E=[128, SEQ], convw=[128, 16], rgvec=[128, 16], BDa=[128, 512], BDx=[128, 512], BDWk=[128, 32 * 128],
        BDWv=[128, 32 * 128], pe2=[128, 64], ov=[128, 4 * 128], gain=[128, 8], wout=[D_MODEL, D_MODEL],
        lnp=[4, D_MODEL], wr=[D_MODEL, 20], rb=[1, 20], wup=[16, D_MODEL, 1024], wdn=[16, 512, D_MODEL],
        wp=[256, D_MODEL], wg=[D_MODEL, D_MODEL])
    _decl = {}

    class _Lazy:
        def __init__(self, name):
            self.name = name

        def _ap(self):
            if self.name not in _decl:
                _decl[self.name] = din(self.name, _shapes[self.name])
            return _decl[self.name]

        def __getitem__(self, key):
            return self._ap()[key]

        @property
        def tensor(self):
            return self._ap().tensor
    nc._used_inputs = _decl
    xs, po, keybias_d, cbias_d, selbias_d, padmask_d = [_Lazy(n) for n in ("xs", "po", "keybias", "cbias", "selbias", "padmask")]
    wA_d, wC_d, tabs_d, cb_d, E_d, convw_d, rgvec_d, BDa_d, BDx_d, BDWk_d, BDWv_d, pe2_d, ov_d, gain_d = [
        _Lazy(n) for n in ("wA", "wC", "tabs", "cb", "E", "convw", "rgvec", "BDa", "BDx", "BDWk", "BDWv", "pe2", "ov", "gain")]
    wout_d, lnp_d, wr_d, rb_d, wup_d, wdn_d, wp_d, wg_d = [_Lazy(n) for n in ("wout", "lnp", "wr", "rb", "wup", "wdn", "wp", "wg")]
    out_d = None if "stop1" in dbg or "stop2a" in dbg or "stop2b" in dbg or "stop2c" in dbg else dout("out", [NOWN * 128, D_MODEL])
    dbg_t = {}
    if "p1" in dbg:
        dbg_t["KsT"] = dout("d_KsT", [128, SEQ])
        dbg_t["KwT"] = dout("d_KwT", [128, SEQ])
        dbg_t["Vs"] = dout("d_Vs", [128, NT * 130])
        dbg_t["KcT"] = dout("d_KcT", [128, 512])
        dbg_t["OVV"] = dout("d_OVV", [128, 4 * 2 * 193])
        dbg_t["hown"] = dout("d_hown", [128, 4 * 2048])
    if "p2a" in dbg:
        dbg_t["QT"] = dout("d_QT", [128, NOWN * 512])
        dbg_t["rnnT"] = dout("d_rnnT", [128, 4 * 2048])
        dbg_t["gates"] = dout("d_gates", [128, NOWN * 24])
        dbg_t["ssr"] = dout("d_ssr", [128, NOWN])
    if "p2b" in dbg:
        dbg_t["attnT"] = dout("d_attnT", [128, 4 * 2048])
        dbg_t["rstda"] = dout("d_rstda", [128, NOWN])
        dbg_t["sel"] = dout("d_sel", [128, NOWN * 2 * 128])
    if "p2c" in dbg:
        dbg_t["x1"] = dout("d_x1", [128, NOWN * 1024])

    with ExitStack() as top:
        def tsb(name, shape, dt):
            return top.enter_context(nc.sbuf_tensor("sb_" + name, list(shape), dt))

        sems = {}
        for k in list(COMPUTE) + [("sp", r) for r in range(NRING)] + [("pool", r) for r in range(NRING)]:
            nm = k if isinstance(k, str) else f"{k[0]}{k[1]}"
            sems[k] = top.enter_context(nc.semaphore("s_" + nm))
        ps = [top.enter_context(nc.psum_tensor(f"ps{k}", [128, 512], F32)) for k in range(8)]
        psb = [p[:].bitcast(BF16) for p in ps]

        ident = tsb("ident", [128, 128], BF16)
        identf = tsb("identf", [128, 128], F32)
        base = {}
        ssr = tsb("ssr", [128, NOWN], F32)
        rstd_a = tsb("rstd_a", [128, NOWN], F32)

        def dump(S, es, name, src_ap, dt):
            d = dbg_t[name]
            n = d.shape[1]
            CH = 1024
            tmp = es.dump_tmp
            for c0 in range(0, n, CH):
                c1 = min(n, c0 + CH)
                S.add("pool", lambda e, c0=c0, c1=c1: e.tensor_copy(out=tmp[:, 0:c1 - c0], in_=src_ap[:, c0:c1]),
                      reads=[], writes=["dmp"])
                S.add("sp", lambda e, c0=c0, c1=c1: e.dma_start(out=d[:, c0:c1], in_=tmp[:, 0:c1 - c0]),
                      reads=["dmp"], writes=[f"dump_{name}_{c0}"], dma=True)

        with ExitStack() as attn_scope:
            def asb(name, shape, dt):
                return attn_scope.enter_context(nc.sbuf_tensor("sb_" + name, list(shape), dt))
            KsT = asb("KsT", [128, SEQ], BF16)
            KwT = asb("KwT", [128, SEQ], BF16)
            Vs = asb("Vs", [128, NT * 130 + 64], BF16)
            Vw = asb("Vw", [128, NT * 130 + 64], BF16)
            KcT = asb("KcT", [128, 512], BF16)
            OVV = asb("OVV", [128, 4 * 2 * 193], BF16)

            with ExitStack() as es:
                def sb(name, shape, dt):
                    return es.enter_context(nc.sbuf_tensor("sb_" + name, list(shape), dt))
                S = Sched(base)
                h_scope = ExitStack()
                hown = h_scope.enter_context(nc.sbuf_tensor("sb_hown", [128, 4 * 2048], BF16))
                wA = sb("wA", [128, 8 * 1280], BF16)
                wst = [sb("wst0", [128, 1280], F32)] * 2
                xst = [sb(f"xst{k}", [128, 1024], F32) for k in range(2)]
                xb = [sb(f"xb{k}", [128, 1024], BF16) for k in range(2)]
                xT = [sb(f"xT{k}", [128, 8 * 512], BF16) for k in range(2)]
                kraw = sb("kraw", [128, 16 + 2048 + 32], BF16)
                vraw = sb("vraw", [128, 16 + 2048 + 32], BF16)
                BDWk = sb("BDWk", [128, 32 * 128], BF16)
                BDWv = sb("BDWv", [128, 32 * 128], BF16)
                pe2 = sb("pe2", [128, 64], F32)
                pe2b = sb("pe2b", [128, 64], BF16)
                cbk = sb("cbk", [128, 2], F32)
                vcs = sb("vcs", [128, 128], BF16)
                convw = sb("convw", [128, 16], F32)
                rgvec = sb("rgvec", [128, 16], F32)
                rgc = sb("rgc", [128, 20], F32)
                dg = sb("dg", [128, 16 * 128], BF16)
                BDa = sb("BDa", [128, 512], BF16)
                BDx = sb("BDx", [128, 512], BF16)
                bdst = sb("bdst", [128, 512], F32)
                rxb = [[sb(f"rxb{p}_{ct}", [128, 515], BF16) for ct in range(4)] for p in range(2)]
                NR = 2
                xcf = [sb(f"xcf{k}", [128, 512], F32) for k in range(NR)]
                xcb = [sb(f"xcb{k}", [128, 512], BF16) for k in range(NR)]
                thr_ = [sb(f"thr{k}", [128, 512], F32) for k in range(NR)]
                thx = [sb(f"thx{k}", [128, 512], F32) for k in range(NR)]
                av = [sb(f"av{k}", [128, 512], F32) for k in range(NR)]
                hb = [sb(f"hb{k}", [128, 512], F32) for k in range(NR)]
                carry = sb("carry", [128, 4], F32)
                padmask = sb("padmask", [128, 512], F32)

                S.add("pool", lambda e: e.memset(identf[:], 0.0), writes=["identf"])
                S.add("pool", lambda e: e.affine_select(out=identf[:], in_=identf[:], compare_op=ALU.not_equal, fill=1.0,
                                                        base=0, pattern=[[-1, 128]], channel_multiplier=1),
                      reads=["identf"], writes=["identf"])
                S.add("pool", lambda e: e.tensor_copy(out=ident[:], in_=identf[:]), reads=["identf"], writes=["ident"])
                S.add("pool", lambda e: e.memset(carry[:], 0.0), writes=["carry"])
                S.add("pool", lambda e: e.memset(kraw[:], 0.0), writes=["kraw"])
                S.add("pool", lambda e: e.memset(vraw[:], 0.0), writes=["vraw"])
                S.add("pool", lambda e: e.memset(Vs[:], 1.0), writes=["Vs"])
                S.add("pool", lambda e: e.memset(Vw[:], 1.0), writes=["Vw"])
                S.add("pool", lambda e: e.memset(OVV[:], 1.0), writes=["OVV"])
                for p in range(2):
                    for ct in range(4):
                        S.add("pool", lambda e, p=p, ct=ct: e.memset(rxb[p][ct][:, 0:3], 0.0), writes=[f"rxb{p}_{ct}"])
                S.add("sp", lambda e: e.dma_start(out=convw[:], in_=convw_d[:, :]), writes=["convw"], dma=True)
                S.add("sp", lambda e: e.dma_start(out=rgvec[:], in_=rgvec_d[:, :]), writes=["rgvec"], dma=True)
                S.add("sp", lambda e: e.dma_start(out=pe2[:], in_=pe2_d[:, :]), writes=["pe2"], dma=True)
                S.add("sp", lambda e: e.dma_start(out=padmask[:], in_=padmask_d[:, :]), writes=["padmask"], dma=True)
                for L0 in range(2):
                    S.add("sp", lambda e, L0=L0: e.dma_start(out=xst[L0][:], in_=xs[L0 * 128:(L0 + 1) * 128, :]), writes=[f"xst{L0}"], dma=True)
                    S.add("dve", lambda e, L0=L0: e.tensor_copy(out=xb[L0][:], in_=xst[L0][:]), reads=[f"xst{L0}"], writes=[f"xb{L0}"])
                stB = xT[1][:].bitcast(F32)[:, 0:1280]
                for dc in range(8):
                    st = wst[0][:] if dc % 2 == 0 else stB
                    sk_ = "wst0" if dc % 2 == 0 else "xT1"
                    S.add("sp", lambda e, dc=dc, st=st: e.dma_start(out=st, in_=wA_d[dc * 128:(dc + 1) * 128, :]),
                          writes=[sk_], dma=True)
                    if dc % 2 == 0:
                        S.add("dve", lambda e, dc=dc, st=st: e.tensor_copy(out=wA[:, dc * 1280:(dc + 1) * 1280], in_=st),
                              reads=[sk_], writes=[f"wA{dc}"])
                    else:
                        S.add("act", lambda e, dc=dc, st=st: e.copy(out=wA[:, dc * 1280:(dc + 1) * 1280], in_=st),
                              reads=[sk_], writes=[f"wA{dc}"])
                wA_keys = [f"wA{dc}" for dc in range(8)]
                for nm, src, dst in (("BDa", BDa_d, BDa), ("BDx", BDx_d, BDx)):
                    S.add("sp", lambda e, src=src: e.dma_start(out=bdst[:], in_=src[:, :]), writes=["bdst"], dma=True)
                    S.add("dve", lambda e, dst=dst: e.tensor_copy(out=dst[:], in_=bdst[:]), reads=["bdst"], writes=[nm])
                def late_cw():
                    for nm, src, dst in (("BDWk", BDWk_d, BDWk), ("BDWv", BDWv_d, BDWv)):
                        for c in range(4):
                            st = wst[c % 2]
                            S.add("sp", lambda e, src=src, c=c, st=st: e.dma_start(out=st[:, 0:1024], in_=src[:, c * 1024:(c + 1) * 1024]),
                                  writes=["wst0"], dma=True)
                            S.add("dve", lambda e, dst=dst, c=c, st=st: e.tensor_copy(out=dst[:, c * 1024:(c + 1) * 1024], in_=st[:, 0:1024]),
                                  reads=["wst0"], writes=[nm])
                    S.add("dve", lambda e: e.tensor_copy(out=pe2b[:], in_=pe2[:]), reads=["pe2"], writes=["pe2b"])
                    S.add("sp", lambda e: e.dma_start(out=bdst[:], in_=ov_d[:, :]), writes=["bdst"], dma=True)
                    for ct in range(4):
                        for g in range(2):
                            o0 = (ct * 2 + g) * 193
                            S.add("dve", lambda e, ct=ct, o0=o0: e.tensor_copy(out=OVV[:, o0:o0 + 128], in_=bdst[:, ct * 128:(ct + 1) * 128]),
                                  reads=["bdst", "OVV"], writes=["OVV"])

                for kk in range(16):
                    S.add("dve", lambda e, kk=kk: e.tensor_scalar(out=dg[:, kk * 128:(kk + 1) * 128], in0=identf[:],
                                                                    scalar1=convw[:, kk:kk + 1], scalar2=None, op0=ALU.mult),
                          reads=["identf", "convw"], writes=["dg"])
                S.add("dve", lambda e: e.tensor_scalar(out=rgc[:, 0:8], in0=rgvec[:, 4:12], scalar1=0.5, scalar2=None, op0=ALU.mult),
                      reads=["rgvec"], writes=["rgc"])
                S.add("act", lambda e: e.activation(out=rgc[:, 16:20], in_=rgvec[:, 12:16], func=AF.Exp, scale=-1.0),
                      reads=["rgvec", "rgc"], writes=["rgc"])
                S.add("act", lambda e: e.activation(out=rgc[:, 16:20], in_=rgc[:, 16:20], func=AF.Ln, bias=1.0, scale=1.0),
                      reads=["rgc"], writes=["rgc"])
                S.add("dve", lambda e: e.tensor_scalar(out=rgc[:, 8:12], in0=rgc[:, 16:20], scalar1=-8.0, scalar2=None, op0=ALU.mult),
                      reads=["rgc"], writes=["rgc"])
                S.add("dve", lambda e: e.tensor_scalar(out=rgc[:, 12:16], in0=rgc[:, 16:20], scalar1=-4.0, scalar2=None, op0=ALU.mult),
                      reads=["rgc"], writes=["rgc"])
                def late_cb():
                    for w_, (nm, W) in enumerate((("BDWk", BDWk), ("BDWv", BDWv))):
                        for l in range(32):
                            S.add("pe", lambda e, W=W, l=l, w_=w_: e.matmul(ps[7][:, w_:w_ + 1], lhsT=W[:, l * 128:(l + 1) * 128],
                                                                             rhs=pe2b[:, w_ * 32 + l:w_ * 32 + l + 1],
                                                                             start=(l == 0), stop=(l == 31)),
                                  reads=[nm, "pe2b"], writes=["ps7"])
                    S.add("dve", lambda e: e.tensor_copy(out=cbk[:], in_=ps[7][:, 0:2]), reads=["ps7"], writes=["cbk"])


                def load_tile(L):
                    k = L % 2
                    S.add("sp", lambda e: e.dma_start(out=xst[k][:], in_=xs[L * 128:(L + 1) * 128, :]), writes=[f"xst{k}"], dma=True)
                    S.add("dve", lambda e: e.tensor_copy(out=xb[k][:], in_=xst[k][:]), reads=[f"xst{k}"], writes=[f"xb{k}"])

                def transpose_tile(L):
                    k = L % 2
                    G, t = divmod(L, 4)
                    pb = psb[k]
                    for dc in range(8):
                        S.add("pe", lambda e, dc=dc: e.transpose(out=pb[:, dc * 128:(dc + 1) * 128], in_=xb[k][:, dc * 128:(dc + 1) * 128], identity=ident[:]),
                              reads=[f"xb{k}", "ident"], writes=[f"ps{k}"])
                    dst = xT[G % 2][:].rearrange("p (c n) -> p c n", c=8)[:, :, t * 128:(t + 1) * 128]
                    src = pb[:].rearrange("p (c n) -> p c n", c=8)
                    if L % 2 == 0:
                        S.add("dve", lambda e: e.tensor_copy(out=dst, in_=src), reads=[f"ps{k}"], writes=[f"xT{G % 2}"])
                    else:
                        S.add("act", lambda e: e.copy(out=dst, in_=src), reads=[f"ps{k}"], writes=[f"xT{G % 2}"])

                def vproj_tile(L):
                    G, t = divmod(L, 4)
                    X = xT[G % 2]
                    for dc in range(8):
                        S.add("pe", lambda e, dc=dc: e.matmul(ps[2][:, 0:256], lhsT=X[:, dc * 512 + t * 128: dc * 512 + (t + 1) * 128],
                                                               rhs=wA[:, dc * 1280 + 1024: dc * 1280 + 1280], start=(dc == 0), stop=(dc == 7)),
                              reads=[f"xT{G % 2}", f"wA{dc}"], writes=["ps2"])
                    for vi, (nm, V) in enumerate((("Vs", Vs), ("Vw", Vw))):
                        dst = V[:, L * 130:(L + 1) * 130].rearrange("p (g c) -> p g c", g=2)[:, :, 0:64]
                        src = ps[2][:, vi * 128:(vi + 1) * 128].rearrange("p (g c) -> p g c", g=2)
                        S.add("dve", lambda e, dst=dst, src=src: e.tensor_copy(out=dst, in_=src), reads=["ps2"], writes=[nm])

                def fproj(G, m):
                    X = xT[G % 2]
                    bank = 3 + (m % 2)
                    for dc in range(8):
                        S.add("pe", lambda e, dc=dc: e.matmul(ps[bank][:, :], lhsT=wA[:, dc * 1280 + m * 128: dc * 1280 + (m + 1) * 128],
                                                               rhs=X[:, dc * 512:(dc + 1) * 512], start=(dc == 0), stop=(dc == 7)),
                              reads=[f"xT{G % 2}", f"wA{dc}"], writes=[f"ps{bank}"])
                    if m == 0:
                        o = 16 + (G % 4) * 512
                        S.add("act", lambda e: e.copy(out=kraw[:, o:o + 512], in_=ps[bank][:, :]), reads=[f"ps{bank}"], writes=["kraw"])
                    elif m == 1:
                        o = 16 + (G % 4) * 512
                        S.add("act", lambda e: e.copy(out=vraw[:, o:o + 512], in_=ps[bank][:, :]), reads=[f"ps{bank}"], writes=["vraw"])
                    elif m == 2:
                        S.add("dve", lambda e: e.tensor_copy(out=KsT[:, G * 512:(G + 1) * 512], in_=ps[bank][:, :]), reads=[f"ps{bank}"], writes=["KsT"])
                    elif m == 3:
                        S.add("dve", lambda e: e.tensor_copy(out=KwT[:, G * 512:(G + 1) * 512], in_=ps[bank][:, :]), reads=[f"ps{bank}"], writes=["KwT"])
                    else:
                        ct = m - 4
                        p = G % 2
                        if G > 0:
                            S.add("pool", lambda e: e.tensor_copy(out=rxb[p][ct][:, 0:3], in_=rxb[1 - p][ct][:, 512:515]),
                                  reads=[f"rxb{1 - p}_{ct}"], writes=[f"rxb{p}_{ct}"])
                        S.add("act", lambda e: e.copy(out=rxb[p][ct][:, 3:515], in_=ps[bank][:, :]), reads=[f"ps{bank}"], writes=[f"rxb{p}_{ct}"])

                def rnn_conv(G, ct):
                    p = G % 2
                    for k in range(4):
                        S.add("pe", lambda e, k=k: e.matmul(ps[5][:, :], lhsT=dg[:, (k * 4 + ct) * 128:(k * 4 + ct + 1) * 128],
                                                             rhs=rxb[p][ct][:, k:k + 512], start=(k == 0), stop=(k == 3)),
                              reads=["dg", f"rxb{p}_{ct}"], writes=["ps5"])
                    r = ct % NR
                    S.add("act", lambda e: e.activation(out=xcb[r][:], in_=ps[5][:, :], func=AF.Identity, bias=rgvec[:, ct:ct + 1], scale=1.0),
                          reads=["ps5", "rgvec"], writes=[f"xcb{r}"])
                    S.add("act", lambda e: e.activation(out=xcf[r][:], in_=ps[5][:, :], func=AF.Identity, bias=rgvec[:, ct:ct + 1], scale=1.0),
                          reads=["ps5", "rgvec"], writes=[f"xcf{r}"])

                def rnn_gates_front(G, cts):
                    for ct in cts:
                        r = ct % NR
                        S.add("pe", lambda e, ct=ct, r=r: e.matmul(ps[6][:, :], lhsT=BDa[:, ct * 128:(ct + 1) * 128], rhs=xcb[r][:], start=True, stop=True),
                              reads=["BDa", f"xcb{r}"], writes=["ps6"])
                        S.add("pe", lambda e, ct=ct, r=r: e.matmul(ps[7][:, :], lhsT=BDx[:, ct * 128:(ct + 1) * 128], rhs=xcb[r][:], start=True, stop=True),
                              reads=["BDx", f"xcb{r}"], writes=["ps7"])
                        S.add("act", lambda e, ct=ct, r=r: e.activation(out=thr_[r][:], in_=ps[6][:, :], func=AF.Tanh, bias=rgc[:, ct:ct + 1], scale=0.5),
                              reads=["ps6", "rgc"], writes=[f"thr{r}"])
                        S.add("act", lambda e, ct=ct, r=r: e.activation(out=thx[r][:], in_=ps[7][:, :], func=AF.Tanh, bias=rgc[:, 4 + ct:5 + ct], scale=0.5),
                              reads=["ps7", "rgc"], writes=[f"thx{r}"])

                def rnn_gates(G, cts):
                    for ct in cts:
                        r = ct % NR
                        S.add("act", lambda e, ct=ct, r=r: e.activation(out=av[r][:], in_=thr_[r][:], func=AF.Exp, bias=rgc[:, 12 + ct:13 + ct], scale=rgc[:, 12 + ct:13 + ct]),
                              reads=[f"thr{r}", "rgc"], writes=[f"av{r}"])
                        S.add("act", lambda e, ct=ct, r=r: e.activation(out=thr_[r][:], in_=thr_[r][:], func=AF.Exp, bias=rgc[:, 8 + ct:9 + ct], scale=rgc[:, 8 + ct:9 + ct]),
                              reads=[f"thr{r}", "rgc"], writes=[f"thr{r}"])
                    for ct in cts:
                        r = ct % NR
                        S.add("act", lambda e, r=r: e.activation(out=thr_[r][:], in_=thr_[r][:], func=AF.Sqrt, bias=1.0, scale=-1.0),
                              reads=[f"thr{r}"], writes=[f"thr{r}"])
                    for ct in cts:
                        r = ct % NR
                        S.add("dve", lambda e, r=r: e.scalar_tensor_tensor(out=thx[r][:], in0=thx[r][:], scalar=1.0, in1=xcf[r][:], op0=ALU.add, op1=ALU.mult),
                              reads=[f"thx{r}", f"xcf{r}"], writes=[f"thx{r}"])

                def rnn_scan(G, cts):
                    for ct in cts:
                        r = ct % NR
                        S.add("dve", lambda e, r=r: e.scalar_tensor_tensor(out=thx[r][:], in0=thr_[r][:], scalar=0.5, in1=thx[r][:], op0=ALU.mult, op1=ALU.mult),
                              reads=[f"thx{r}", f"thr{r}"], writes=[f"thx{r}"])
                        if G == 0:
                            S.add("dve", lambda e, r=r: e.tensor_tensor(out=thx[r][:], in0=thx[r][:], in1=padmask[:], op=ALU.mult),
                                  reads=[f"thx{r}", "padmask"], writes=[f"thx{r}"])
                        S.add("dve", lambda e, ct=ct, r=r: e.tensor_tensor_scan(out=hb[r][:], data0=av[r][:], data1=thx[r][:], initial=carry[:, ct:ct + 1],
                                                                            op0=ALU.mult, op1=ALU.add),
                              reads=[f"av{r}", f"thx{r}", "carry"], writes=[f"hb{r}"])
                        S.add("dve", lambda e, ct=ct, r=r: e.tensor_copy(out=carry[:, ct:ct + 1], in_=hb[r][:, 511:512]), reads=[f"hb{r}", "carry"], writes=["carry"])
                        S.add("pool", lambda e, ct=ct, r=r: e.tensor_copy(out=hown[:, ct * 2048 + G * 128: ct * 2048 + (G + 1) * 128], in_=hb[r][:, 384:512]),
                              reads=[f"hb{r}"], writes=["hown"])

                def compress(SG):
                    for w_, (nm, W, raw, rawk) in enumerate((("BDWk", BDWk, kraw, "kraw"), ("BDWv", BDWv, vraw, "vraw"))):
                        bank = 3 + w_
                        for l in range(32):
                            S.add("pe", lambda e, W=W, raw=raw, l=l, bank=bank: e.matmul(ps[bank][:, 0:128], lhsT=W[:, l * 128:(l + 1) * 128],
                                                                                       rhs=raw[:, l:l + 2048:16], start=(l == 0), stop=(l == 31)),
                                  reads=[nm, rawk], writes=[f"ps{bank}"])
                        if w_ == 0:
                            S.add("act", lambda e: e.activation(out=KcT[:, SG * 128:(SG + 1) * 128], in_=ps[3][:, 0:128], func=AF.Identity,
                                                                bias=cbk[:, 0:1], scale=1.0),
                                  reads=["ps3", "cbk"], writes=["KcT"])
                        else:
                            S.add("act", lambda e: e.activation(out=vcs[:], in_=ps[4][:, 0:128], func=AF.Identity, bias=cbk[:, 1:2], scale=1.0),
                                  reads=["ps4", "cbk"], writes=["vcs"])
                            S.add("pe", lambda e: e.transpose(out=psb[4][:, 512:640], in_=vcs[:], identity=ident[:]),
                                  reads=["vcs", "ident", "ps4"], writes=["ps4"])
                            dst = OVV[:, SG * 386:(SG + 1) * 386].rearrange("p (g c) -> p g c", g=2)[:, :, 128:192]
                            src = psb[4][:, 512:640].rearrange("p (g c) -> p g c", g=2)
                            S.add("dve", lambda e, dst=dst, src=src: e.tensor_copy(out=dst, in_=src), reads=["ps4", "OVV"], writes=["OVV"])
                        S.add("pool", lambda e, raw=raw: e.tensor_copy(out=raw[:, 0:16], in_=raw[:, 2048:2064]), reads=[rawk], writes=[rawk])

                for t in range(4):
                    transpose_tile(t)
                    load_tile(t + 2)
                late_cw()
                for t in range(4):
                    vproj_tile(t)
                for G in range(18):
                    rn = [None] * 8
                    if 1 <= G <= 16:
                        rn = [("c", 0), ("c", 1), ("f", (0,)), ("fg", ((1,), (0, 1))), ("c", 2), ("cs", (3, (0, 1))), ("f", (2,)), ("fg", ((3,), (2, 3)))]
                    for m in range(8):
                        if G < 16:
                            fproj(G, m)
                        if m == 1 and G >= 2:
                            rnn_scan(G - 2, (2, 3))
                        if rn[m] is not None:
                            kind, ct = rn[m]
                            if kind == "c":
                                rnn_conv(G - 1, ct)
                            elif kind == "cs":
                                rnn_conv(G - 1, ct[0])
                                rnn_scan(G - 1, ct[1])
                            elif kind == "f":
                                rnn_gates_front(G - 1, ct)
                            else:
                                rnn_gates_front(G - 1, ct[0])
                                rnn_gates(G - 1, ct[1])
                        if G + 1 < 16:
                            L = 4 * (G + 1) + m // 2
                            if m % 2 == 0:
                                transpose_tile(L)
                                if L + 2 < NT:
                                    load_tile(L + 2)
                            else:
                                vproj_tile(L)
                    if G == 0:
                        late_cb()
                    if G < 16 and G % 4 == 3:
                        compress(G // 4)
                if "p1" in dbg:
                    es.dump_tmp = wst[0]
                    dump(S, es, "KsT", KsT, BF16)
                    dump(S, es, "KwT", KwT, BF16)
                    dump(S, es, "Vs", Vs, BF16)
                    dump(S, es, "KcT", KcT, BF16)
                    dump(S, es, "OVV", OVV, BF16)
                    dump(S, es, "hown", hown, BF16)
                S.drain()
                with nc.Block() as block:
                    base = S.emit(block, sems)
            if "stop1" in dbg:
                h_scope.close()
                return nc

            r_scope = ExitStack()

            def rsb(name, shape, dt):
                return r_scope.enter_context(nc.sbuf_tensor("sb_" + name, list(shape), dt, side="right"))
            rnnT = rsb("rnnT", [128, 4 * 2048], BF16)
            gates = rsb("gates", [128, NOWN * 24], F32)
            attnT = rsb("attnT", [128, 4 * 2048], BF16)
            q_scope = ExitStack()
            QT = [q_scope.enter_context(nc.sbuf_tensor(f"sb_QT{g}", [128, NOWN * 512], BF16, side="right")) for g in range(2)]

            with ExitStack() as es:
                def sb(name, shape, dt):
                    return es.enter_context(nc.sbuf_tensor("p2a_" + name, list(shape), dt))
                S = Sched(base)
                wC = sb("wC", [128, 8 * 1048], BF16)
                wst = sb("wst2", [128, 1048], F32)
                wstb = sb("wst2b", [128, 1048], F32)
                xst = [sb(f"xst{k}", [128, 1024], F32) for k in range(2)]
                xb = [sb(f"xb{k}", [128, 1024], BF16) for k in range(2)]
                xTo = [sb("xTo0", [128, 8 * 512], BF16)] * 2
                gel = [sb(f"gel{k}", [128, 512], F32) for k in range(2)]
                gf = sb("gf", [128, 4 * 512], F32)
                onesf = sb("onesf", [128, 1], F32)
                gtmp = sb("gtmp", [128, 24], F32)
                S.add("pool", lambda e: e.memset(onesf[:], 1.0), writes=["onesf"])
                for g in range(2):
                    S.add("pool", lambda e, g=g: e.memset(QT[g][:], 0.0), writes=["QT"])
                for dc in range(8):
                    wb = wst if dc % 2 == 0 else wstb
                    S.add("sp", lambda e, dc=dc, wb=wb: e.dma_start(out=wb[:], in_=wC_d[dc * 128:(dc + 1) * 128, :]), writes=[f"wst{dc % 2}"], dma=True)
                    S.add("dve" if dc % 2 == 0 else "pool", lambda e, dc=dc, wb=wb: e.tensor_copy(out=wC[:, dc * 1048:(dc + 1) * 1048], in_=wb[:]), reads=[f"wst{dc % 2}"], writes=[f"wC{dc}"])
                wC_keys = [f"wC{dc}" for dc in range(8)]

                def load_own(i):
                    k = i % 2
                    L = 4 * i + 3
                    S.add("sp", lambda e: e.dma_start(out=xst[k][:], in_=xs[L * 128:(L + 1) * 128, :]), writes=[f"xst{k}"], dma=True)
                    S.add("dve", lambda e: e.tensor_copy(out=xb[k][:], in_=xst[k][:]), reads=[f"xst{k}"], writes=[f"xb{k}"])

                load_own(0)
                load_own(1)
                def do_I(I):
                    X = xTo[I % 2]
                    for t in range(4):
                        i = 4 * I + t
                        k = i % 2
                        pb = psb[k]
                        for dc in range(8):
                            S.add("pe", lambda e, dc=dc, k=k, pb=pb: e.transpose(out=pb[:, dc * 128:(dc + 1) * 128], in_=xb[k][:, dc * 128:(dc + 1) * 128], identity=ident[:]),
                                  reads=[f"xb{k}", "ident"], writes=[f"ps{k}"])
                        dst = X[:].rearrange("p (c n) -> p c n", c=8)[:, :, t * 128:(t + 1) * 128]
                        src = pb[:].rearrange("p (c n) -> p c n", c=8)
                        S.add("act", lambda e, dst=dst, src=src: e.copy(out=dst, in_=src), reads=[f"ps{k}"], writes=["xTo0"])
                        if i + 2 < NOWN:
                            load_own(i + 2)
                    for t in range(4):
                        i = 4 * I + t
                        for dc in range(8):
                            S.add("pe", lambda e, dc=dc, t=t: e.matmul(ps[2][:, 0:24], lhsT=X[:, dc * 512 + t * 128: dc * 512 + (t + 1) * 128],
                                                                     rhs=wC[:, dc * 1048 + 1024: dc * 1048 + 1048], start=(dc == 0), stop=(dc == 7)),
                                  reads=["xTo0", f"wC{dc}"], writes=["ps2"])
                        S.add("act", lambda e: e.activation(out=gtmp[:], in_=ps[2][:, 0:24], func=AF.Tanh, scale=0.5), reads=["ps2"], writes=["gtmp"])
                        S.add("dve", lambda e, i=i: e.tensor_scalar(out=gates[:, i * 24:(i + 1) * 24], in0=gtmp[:], scalar1=0.5, scalar2=0.5, op0=ALU.mult, op1=ALU.add),
                              reads=["gtmp"], writes=["gates"])
                    for m in range(8):
                        bank = 3 + (m % 2)
                        for dc in range(8):
                            S.add("pe", lambda e, dc=dc, m=m, bank=bank: e.matmul(ps[bank][:, :], lhsT=wC[:, dc * 1048 + m * 128: dc * 1048 + (m + 1) * 128],
                                                                               rhs=X[:, dc * 512:(dc + 1) * 512], start=(dc == 0), stop=(dc == 7)),
                                  reads=["xTo0", f"wC{dc}"], writes=[f"ps{bank}"])
                        if m < 4:
                            for g in range(2):
                                gpp = slice(64 * g, 64 * g + 64)
                                dst = QT[g][gpp, 4 * I * 512:(4 * I + 4) * 512].rearrange("p (t r q) -> p t r q", t=4, r=4)[:, :, m, :]
                                src = ps[bank][gpp, :].rearrange("p (t q) -> p t q", t=4)
                                S.add("act", lambda e, dst=dst, src=src: e.activation(out=dst, in_=src, func=AF.Copy, scale=0.125), reads=[f"ps{bank}"], writes=["QT"])
                        else:
                            ct = m - 4
                            gb = gel[ct % 2]
                            S.add("act", lambda e, gb=gb, bank=bank: e.activation(out=gb[:], in_=ps[bank][:, :], func=AF.Gelu_apprx_tanh),
                                  reads=[f"ps{bank}"], writes=[f"gel{ct % 2}"])
                            S.add("dve", lambda e, gb=gb, ct=ct: e.tensor_tensor(out=gf[:, ct * 512:(ct + 1) * 512], in0=gb[:],
                                                                               in1=hown[:, ct * 2048 + I * 512: ct * 2048 + (I + 1) * 512], op=ALU.mult),
                                  reads=[f"gel{ct % 2}", "hown"], writes=[f"gf{ct}"])
                            S.add("act", lambda e, ct=ct: e.copy(out=rnnT[:, ct * 2048 + I * 512: ct * 2048 + (I + 1) * 512], in_=gf[:, ct * 512:(ct + 1) * 512]),
                                  reads=[f"gf{ct}"], writes=["rnnT"])
                            S.add("pool", lambda e, ct=ct: e.tensor_tensor(out=gf[:, ct * 512:(ct + 1) * 512], in0=gf[:, ct * 512:(ct + 1) * 512],
                                                                          in1=gf[:, ct * 512:(ct + 1) * 512], op=ALU.mult),
                                  reads=[f"gf{ct}"], writes=[f"gf{ct}"])
                    for t in range(4):
                        i = 4 * I + t
                        for ct in range(4):
                            S.add("pe", lambda e, i=i, t=t, ct=ct: e.matmul(ps[5][:, i:i + 1], lhsT=gf[:, ct * 512 + t * 128: ct * 512 + (t + 1) * 128],
                                                                         rhs=onesf[:, 0:1], start=(ct == 0), stop=(ct == 3)),
                                  reads=[f"gf{ct}", "onesf"], writes=["ps5"])
                for I in range(4):
                    do_I(I)
                S.add("dve", lambda e: e.tensor_copy(out=ssr[:], in_=ps[5][:, 0:NOWN]), reads=["ps5"], writes=["ssr"])
                if "p2a" in dbg:
                    es.dump_tmp = wst
                    dump(S, es, "QT", QT[0], BF16)
                    dump(S, es, "rnnT", rnnT, BF16)
                    dump(S, es, "gates", gates, F32)
                    dump(S, es, "ssr", ssr, F32)
                S.drain()
                with nc.Block() as block:
                    base = S.emit(block, sems)
            h_scope.close()
            if "stop2a" in dbg:
                q_scope.close()
                r_scope.close()
                return nc

            with ExitStack() as es:
                def sb(name, shape, dt):
                    return es.enter_context(nc.sbuf_tensor("p2b_" + name, list(shape), dt))
                S = Sched(base)
                tabs = sb("tabs", [128, NTAB * 1024], BF16)
                Eb = sb("Eb", [128, SEQ], BF16)
                stg = sb("stg", [128, 2048], F32)
                keybias = sb("keybias", [128, NT], F32)
                cbias = sb("cbias", [128, 4], F32)
                cb = sb("cb", [128, 8], F32)
                cbm = sb("cbm", [128, 8], F32)
                selb = [sb(f"selb{k}", [128, 128], F32) for k in range(2)]
                Pc = [sb(f"Pc{k}", [128, 512], BF16) for k in range(4)]
                NP = 4
                Pp = [sb(f"Pp{k}", [128, 512], BF16) for k in range(NP)]
                R0 = [sb(f"R0_{g}", [128, 512], BF16) for g in range(2)]
                Rc = [sb(f"Rc_{g}", [128, 512], BF16) for g in range(2)]
                score = sb("score", [128, 128], F32)
                work = sb("work", [128, 128], F32)
                m16 = sb("m16", [128, 16], F32)
                selm = sb("selm", [128, 128], BF16)
                zr = sb("zr", [128, 4], F32)
                coef = sb("coef", [128, 4], F32)
                OTs = [sb(f"OTs{k}", [128, 512], F32) for k in range(2)]
                attn = [sb(f"attn{k}", [128, 512], F32) for k in range(2)]
                attnb = sb("attnb", [128, 512], BF16)
                junk = sb("junk", [128, 512], BF16)
                ssa = sb("ssa", [128, 2], F32)
                nhalf = sb("nhalf", [128, 1], F32)
                seldbg = sb("seldbg", [128, NOWN * 2 * 128], BF16) if "p2b" in dbg else None

                S.add("pool", lambda e: e.memset(nhalf[:], -0.5), writes=["nhalf"])
                S.add("sp", lambda e: e.dma_start(out=keybias[:], in_=keybias_d[:, :]), writes=["keybias"], dma=True)
                S.add("sp", lambda e: e.dma_start(out=cbias[:], in_=cbias_d[:, :]), writes=["cbias"], dma=True)
                S.add("sp", lambda e: e.dma_start(out=cb[:], in_=cb_d[:, :]), writes=["cb"], dma=True)
                S.add("dve", lambda e: e.tensor_scalar(out=cbm[:], in0=cb[:], scalar1=MASKV, scalar2=None, op0=ALU.add), reads=["cb"], writes=["cbm"])
                for tb in range(NTAB):
                    hh = tb % 2
                    S.add("sp", lambda e, tb=tb, hh=hh: e.dma_start(out=stg[:, hh * 1024:(hh + 1) * 1024], in_=tabs_d[tb, :, :]), writes=[f"stg{hh}"], dma=True)
                    S.add("pool" if hh == 0 else "dve", lambda e, tb=tb, hh=hh: e.tensor_copy(out=tabs[:, tb * 1024:(tb + 1) * 1024], in_=stg[:, hh * 1024:(hh + 1) * 1024]),
                          reads=[f"stg{hh}"], writes=["tabs"])
                for c in range(8):
                    hh = c % 2
                    S.add("sp", lambda e, c=c, hh=hh: e.dma_start(out=stg[:, hh * 1024:(hh + 1) * 1024], in_=E_d[:, c * 1024:(c + 1) * 1024]), writes=[f"stg{hh}"], dma=True)
                    S.add("pool" if hh == 0 else "dve", lambda e, c=c, hh=hh: e.tensor_copy(out=Eb[:, c * 1024:(c + 1) * 1024], in_=stg[:, hh * 1024:(hh + 1) * 1024]),
                          reads=[f"stg{hh}"], writes=["Eb"])

                sctr = [0]
                pctr = [0]

                SB = (0, 1, 7)

                def run_jobs(jobs):
                    LAG = 2
                    q = []
                    for job in jobs + [None] * LAG:
                        if job is not None:
                            sb_ = SB[sctr[0] % 3]
                            sctr[0] += 1
                            pp = pctr[0] % NP
                            pctr[0] += 1
                            n = len(job["mms"])
                            for q_, (lh, rh, rds) in enumerate(job["mms"]):
                                S.add("pe", lambda e, lh=lh, rh=rh, q_=q_, n=n, sb_=sb_: e.matmul(ps[sb_][:, :], lhsT=lh, rhs=rh, start=(q_ == 0), stop=(q_ == n - 1)),
                                      reads=rds, writes=[f"ps{sb_}"])
                            S.add("act", lambda e, sb_=sb_, pp=pp, bias=job["bias"]: e.activation(out=Pp[pp][:], in_=ps[sb_][:, :], func=AF.Exp, bias=bias, scale=1.0),
                                  reads=[f"ps{sb_}", "keybias"], writes=[f"Pp{pp}"])
                            job["pp"] = pp
                        q.append(job)
                        if len(q) > LAG:
                            pend = q.pop(0)
                            if pend is not None:
                                ob = pend["obank"]
                                S.add("pe", lambda e, pend=pend, ob=ob: e.matmul(ps[ob][:, :], lhsT=pend["v"], rhs=Pp[pend["pp"]][:], start=pend["first"], stop=pend["last"]),
                                      reads=[f"Pp{pend['pp']}", pend["vkey"]], writes=[f"ps{ob}"])

                def fin_copy(obank, bri):
                    o = OTs[bri % 2]
                    S.add("dve", lambda e: e.tensor_copy(out=o[0:65, :], in_=ps[obank][0:65, :]), reads=[f"ps{obank}"], writes=[f"OTs{bri % 2}"])

                def fin_late(i, g, bri, A):
                    o = OTs[bri % 2]
                    for r in range(4):
                        S.add("pe", lambda e, r=r: e.transpose(out=ps[6][:, r * 65:(r + 1) * 65], in_=o[0:65, r * 128:(r + 1) * 128], identity=identf[0:65, 0:65]),
                              reads=[f"OTs{bri % 2}", "identf"], writes=["ps6"])
                    S.add("dve", lambda e: e.tensor_copy(out=zr[:], in_=ps[6][:, 64:260:65]), reads=["ps6"], writes=["zr"])
                    S.add("dve", lambda e: e.reciprocal(out=zr[:], in_=zr[:]), reads=["zr"], writes=["zr"])
                    g0 = i * 24 + g * 12 + bri
                    S.add("dve", lambda e: e.tensor_tensor(out=coef[:], in0=zr[:], in1=gates[:, g0:g0 + 10:3], op=ALU.mult), reads=["zr", "gates"], writes=["coef"])
                    for r in range(4):
                        h = 4 * g + r
                        S.add("dve", lambda e, r=r, h=h: e.scalar_tensor_tensor(out=A[:, h * 64:(h + 1) * 64], in0=ps[6][:, r * 65:r * 65 + 64], scalar=coef[:, r:r + 1],
                                                                               in1=A[:, h * 64:(h + 1) * 64], op0=ALU.mult, op1=ALU.add),
                              reads=["ps6", "coef", f"attn{i % 2}"], writes=[f"attn{i % 2}"])

                def stage_C1(i, g, A, sbk):
                    if True:
                        Qi = QT[g][:, i * 512:(i + 1) * 512]
                        nct = i // 4 + 1
                        for ct in range(nct):
                            sb_ = SB[sctr[0] % 3]
                            sctr[0] += 1
                            tid = (T_N0 + i % 4) if ct == nct - 1 else T_C
                            S.add("pe", lambda e, ct=ct, sb_=sb_: e.matmul(ps[sb_][:, :], lhsT=KcT[:, ct * 128:(ct + 1) * 128], rhs=Qi, start=True, stop=False),
                                  reads=["KcT", "QT"], writes=[f"ps{sb_}"])
                            S.add("pe", lambda e, tid=tid, sb_=sb_: e.matmul(ps[sb_][:, :], lhsT=ident[:, :], rhs=tabs[:, tid * 1024 + g * 512: tid * 1024 + (g + 1) * 512],
                                                                          start=False, stop=True),
                                  reads=["ident", "tabs"], writes=[f"ps{sb_}"])
                            S.add("act", lambda e, ct=ct, sb_=sb_: e.activation(out=Pc[ct][:], in_=ps[sb_][:, :], func=AF.Exp, bias=cbias[:, ct:ct + 1], scale=1.0),
                                  reads=[f"ps{sb_}", "cbias"], writes=[f"Pc{ct}"])
                        for r in range(4):
                            ub = 2 + r // 2
                            c0 = (r % 2) * 193
                            for ct in range(nct):
                                S.add("pe", lambda e, r=r, ct=ct, ub=ub, c0=c0: e.matmul(ps[ub][:, c0:c0 + 193], lhsT=Pc[ct][:, r * 128:(r + 1) * 128],
                                                                                     rhs=OVV[:, (ct * 2 + g) * 193:(ct * 2 + g + 1) * 193], start=(ct == 0), stop=(ct == nct - 1)),
                                      reads=[f"Pc{ct}", "OVV"], writes=[f"ps{ub}"])
                        for hb_ in range(2):
                            S.add("dve", lambda e, hb_=hb_: e.tensor_scalar(out=zr[:, 2 * hb_:2 * hb_ + 2], in0=ps[2 + hb_][:, 192:386:193], scalar1=1e-30, scalar2=None, op0=ALU.max),
                                  reads=[f"ps{2 + hb_}", "zr"], writes=["zr"])
                        S.add("dve", lambda e: e.reciprocal(out=zr[:], in_=zr[:]), reads=["zr"], writes=["zr"])
                        for r in range(4):
                            ub = 2 + r // 2
                            c0 = (r % 2) * 193
                            if r == 0:
                                S.add("dve", lambda e, ub=ub, c0=c0: e.tensor_scalar(out=score[:], in0=ps[ub][:, c0:c0 + 128], scalar1=zr[:, 0:1], scalar2=None, op0=ALU.mult),
                                      reads=[f"ps{ub}", "zr"], writes=["score"])
                            else:
                                S.add("dve", lambda e, ub=ub, c0=c0, r=r: e.scalar_tensor_tensor(out=score[:], in0=ps[ub][:, c0:c0 + 128], scalar=zr[:, r:r + 1], in1=score[:],
                                                                                             op0=ALU.mult, op1=ALU.add),
                                      reads=[f"ps{ub}", "zr", "score"], writes=["score"])
                        S.add("dve", lambda e, sbk=sbk: e.tensor_tensor(out=score[:], in0=score[:], in1=sbk[:], op=ALU.add), reads=["score", f"selb{i % 2}"], writes=["score"])
                        g0 = i * 24 + g * 12
                        S.add("dve", lambda e, g0=g0: e.tensor_tensor(out=coef[:], in0=zr[:], in1=gates[:, g0:g0 + 10:3], op=ALU.mult), reads=["zr", "gates"], writes=["coef"])
                        for r in range(4):
                            ub = 2 + r // 2
                            c0 = (r % 2) * 193
                            h = 4 * g + r
                            S.add("dve", lambda e, ub=ub, c0=c0, r=r, h=h: e.tensor_scalar(out=A[:, h * 64:(h + 1) * 64], in0=ps[ub][:, c0 + 128:c0 + 192], scalar1=coef[:, r:r + 1],
                                                                                       scalar2=None, op0=ALU.mult),
                                  reads=[f"ps{ub}", "coef"], writes=[f"attn{i % 2}"])
                        S.add("dve", lambda e: e.max(out=m16[:, 0:8], in_=score[:]), reads=["score"], writes=["m16"])
                        S.add("dve", lambda e: e.match_replace(out=work[:], in_to_replace=m16[:, 0:8], in_values=score[:], imm_value=-3.0e38), reads=["score", "m16"], writes=["work"])
                        S.add("dve", lambda e: e.max(out=m16[:, 8:16], in_=work[:]), reads=["work", "m16"], writes=["m16"])
                        S.add("dve", lambda e: e.tensor_scalar(out=selm[:], in0=score[:], scalar1=m16[:, 15:16], scalar2=None, op0=ALU.is_ge), reads=["score", "m16"], writes=["selm"])
                        if seldbg is not None:
                            S.add("pool", lambda e, i=i, g=g: e.tensor_copy(out=seldbg[:, (i * 2 + g) * 128:(i * 2 + g + 1) * 128], in_=selm[:]), reads=["selm"], writes=["seldbg"])

                def stage_C2(i, g):
                    if True:
                        S.add("pe", lambda e: e.transpose(out=psb[6][:, 0:128], in_=selm[:], identity=ident[:]), reads=["selm", "ident"], writes=["ps6"])
                        for r in range(4):
                            h = 4 * g + r
                            S.add("dve", lambda e, r=r, h=h: e.tensor_scalar(out=Rc[g][:, r * 128:(r + 1) * 128], in0=psb[6][:, 0:128], scalar1=-MASKV, scalar2=cbm[:, h:h + 1],
                                                                           op0=ALU.mult, op1=ALU.add),
                                  reads=["ps6", "cbm"], writes=[f"Rc{g}"])
                            S.add("dve", lambda e, r=r: e.tensor_scalar(out=R0[g][:, r * 128:(r + 1) * 128], in0=psb[6][:, 0:128], scalar1=-MASKV, scalar2=MASKV,
                                                                      op0=ALU.mult, op1=ALU.add),
                                  reads=["ps6"], writes=[f"R0{g}"])

                def stage_S(i, g):
                    if True:
                        Qi = QT[g][:, i * 512:(i + 1) * 512]
                        jobs = []
                        Lmax = 4 * i + 3
                        for L in range(Lmax + 1):
                            near = L >= Lmax - 1
                            mms = [(KsT[:, L * 128:(L + 1) * 128], Qi, ["KsT", "QT"]),
                                   (Eb[:, L * 128:(L + 1) * 128], (R0 if near else Rc)[g][:], ["Eb", f"R0{g}" if near else f"Rc{g}"])]
                            if near:
                                tid = T_D if L == Lmax else T_O1
                                mms.append((ident[:, :], tabs[:, tid * 1024 + g * 512: tid * 1024 + (g + 1) * 512], ["ident", "tabs"]))
                            jobs.append(dict(mms=mms, bias=keybias[:, L:L + 1], v=Vs[:, L * 130 + g * 65: L * 130 + g * 65 + 128], vkey="Vs", obank=4,
                                             first=(L == 0), last=(L == Lmax)))
                        run_jobs(jobs)

                def stage_W(i, g):
                    if True:
                        Qi = QT[g][:, i * 512:(i + 1) * 512]
                        Lmax = 4 * i + 3
                        jobs = []
                        Ls = [L for L in range(Lmax - 4, Lmax + 1) if L >= 0]
                        for L in Ls:
                            rel = L - Lmax
                            tid = {0: T_D, -1: T_O1, -2: T_C, -3: T_C, -4: T_W4}[rel]
                            mms = [(KwT[:, L * 128:(L + 1) * 128], Qi, ["KwT", "QT"]),
                                   (ident[:, :], tabs[:, tid * 1024 + g * 512: tid * 1024 + (g + 1) * 512], ["ident", "tabs"])]
                            jobs.append(dict(mms=mms, bias=keybias[:, L:L + 1], v=Vw[:, L * 130 + g * 65: L * 130 + g * 65 + 128], vkey="Vw", obank=5,
                                             first=(L == Ls[0]), last=(L == Lmax)))
                        run_jobs(jobs)

                def block_done(i):
                    A = attn[i % 2]
                    S.add("act", lambda e, A=A: e.activation(out=junk[:], in_=A[:], func=AF.Square, accum_out=ssa[:, 0:1]), reads=[f"attn{i % 2}"], writes=["junk", "ssa"])
                    S.add("dve", lambda e: e.tensor_scalar(out=ssa[:, 1:2], in0=ssa[:, 0:1], scalar1=1.0 / 512.0, scalar2=RMS_EPS, op0=ALU.mult, op1=ALU.add), reads=["ssa"], writes=["ssa2"])
                    S.add("act", lambda e: e.activation(out=ssa[:, 1:2], in_=ssa[:, 1:2], func=AF.Ln), reads=["ssa2"], writes=["ssa2"])
                    S.add("act", lambda e, i=i: e.activation(out=rstd_a[:, i:i + 1], in_=ssa[:, 1:2], func=AF.Exp, scale=-0.5), reads=["ssa2"], writes=["rstd_a"])
                    S.add("pool", lambda e, A=A: e.tensor_copy(out=attnb[:], in_=A[:]), reads=[f"attn{i % 2}"], writes=["attnb"])
                    for fc in range(4):
                        S.add("pe", lambda e, fc=fc: e.transpose(out=psb[6][:, 512 + fc * 128:512 + (fc + 1) * 128], in_=attnb[:, fc * 128:(fc + 1) * 128], identity=ident[:]),
                              reads=["attnb", "ident"], writes=["ps6"])
                    dst = attnT[:].rearrange("p (f n) -> p f n", f=4)[:, :, i * 128:(i + 1) * 128]
                    src = psb[6][:, 512:1024].rearrange("p (f n) -> p f n", f=4)
                    S.add("dve", lambda e, dst=dst, src=src: e.tensor_copy(out=dst, in_=src), reads=["ps6"], writes=["attnT"])

                units = [(i, g) for i in range(NOWN) for g in range(2)]

                def late_S(u):
                    ui, ug = u
                    fin_late(ui, ug, 1, attn[ui % 2])
                    if ug == 1:
                        block_done(ui)

                for n, (i, g) in enumerate(units):
                    A = attn[i % 2]
                    sbk = selb[i % 2]
                    if g == 0:
                        S.add("sp", lambda e, i=i, sbk=sbk: e.dma_start(out=sbk[:], in_=selbias_d[i, :, :]), writes=[f"selb{i % 2}"], dma=True)
                    stage_C1(i, g, A, sbk)
                    stage_W(i, g)
                    if n >= 2:
                        late_S(units[n - 2])
                    if n >= 1:
                        pu = units[n - 1]
                        fin_late(pu[0], pu[1], 2, attn[pu[0] % 2])
                        stage_S(*pu)
                    stage_C2(i, g)
                    if n >= 1:
                        fin_copy(4, 1)
                    fin_copy(5, 2)
                U = units[-1]
                stage_S(*U)
                late_S(units[-2])
                fin_late(U[0], U[1], 2, attn[U[0] % 2])
                fin_copy(4, 1)
                late_S(U)
                if "p2b" in dbg:
                    es.dump_tmp = stg
                    dump(S, es, "attnT", attnT, BF16)
                    dump(S, es, "rstda", rstd_a, F32)
                    dump(S, es, "sel", seldbg, BF16)
                S.drain()
                with nc.Block() as block:
                    base = S.emit(block, sems)
            q_scope.close()
            if "stop2b" in dbg:
                r_scope.close()
                return nc

        with ExitStack() as x_scope:
            def xsb(name, shape, dt):
                return x_scope.enter_context(nc.sbuf_tensor("x_" + name, list(shape), dt))
            x1 = xsb("x1", [128, NOWN * 1024], F32)
            x1T = xsb("x1T", [128, 8 * 2048], BF16)
            comb = xsb("comb", [128, NOWN * 16], F32)
            nhalf16 = xsb("nhalf16", [128, 16], F32)

            with ExitStack() as es:
                def sb(name, shape, dt):
                    return es.enter_context(nc.sbuf_tensor("p2c_" + name, list(shape), dt))
                S = Sched(base)
                wo = sb("wo", [128, 8 * 1024], BF16)
                wst = sb("wst", [128, 1024], F32)
                gain = sb("gain", [128, 8], F32)
                xres = [sb("xres0", [128, 1024], F32)] * 2
                t1 = [sb(f"t1{k}", [128, 1024], F32) for k in range(2)]
                lng = sb("lng", [128, 1024], F32)
                lnb = sb("lnb", [128, 1024], F32)
                stats = sb("stats", [128, 12], F32)
                mv = sb("mv", [128, 2], F32)
                rstd = sb("rstd", [128, 1], F32)
                x1b = [sb(f"x1b{k}", [128, 1024], BF16) for k in range(2)]
                rstd_r = sb("rstd_r", [128, NOWN], F32)
                S.add("pool", lambda e: e.memset(nhalf16[:], -0.5), writes=["nhalf16"])
                S.add("sp", lambda e: e.dma_start(out=gain[:], in_=gain_d[:, :]), writes=["gain"], dma=True)
                S.add("sp", lambda e: e.dma_start(out=lng[:], in_=lnp_d[0:1, :].partition_broadcast(128)), writes=["lng"], dma=True)
                S.add("sp", lambda e: e.dma_start(out=lnb[:], in_=lnp_d[1:2, :].partition_broadcast(128)), writes=["lnb"], dma=True)
                for dc in range(8):
                    S.add("sp", lambda e, dc=dc: e.dma_start(out=wst[:], in_=wout_d[dc * 128:(dc + 1) * 128, :]), writes=["wst"], dma=True)
                    S.add("dve", lambda e, dc=dc: e.tensor_scalar(out=wo[:, dc * 1024:(dc + 1) * 1024], in0=wst[:], scalar1=gain[:, dc:dc + 1], scalar2=None, op0=ALU.mult),
                          reads=["wst", "gain"], writes=[f"wo{dc}"])
                wo_keys = [f"wo{dc}" for dc in range(8)]
                S.add("dve", lambda e: e.tensor_scalar(out=rstd_r[:], in0=ssr[:], scalar1=1.0 / 512.0, scalar2=RMS_EPS, op0=ALU.mult, op1=ALU.add), reads=["ssr"], writes=["rstd_r"])
                S.add("act", lambda e: e.activation(out=rstd_r[:], in_=rstd_r[:], func=AF.Ln), reads=["rstd_r"], writes=["rstd_r"])
                S.add("act", lambda e: e.activation(out=rstd_r[:], in_=rstd_r[:], func=AF.Exp, scale=-0.5), reads=["rstd_r"], writes=["rstd_r"])

                def do_tile_2c(i):
                    k = i % 2
                    L = 4 * i + 3
                    S.add("sp", lambda e: e.dma_start(out=xres[k][:], in_=xs[L * 128:(L + 1) * 128, :]), writes=["xres0"], dma=True)
                    S.add("act", lambda e: e.activation(out=xres[k][:], in_=xres[k][:], func=AF.Copy, scale=ALPHA), reads=["xres0"], writes=["xres0"])
                    for src, sk, b0 in ((attnT, "attnT", 0), (rnnT, "rnnT", 2)):
                        for half in range(2):
                            for fc in range(4):
                                wrow = (fc if b0 == 0 else 4 + fc)
                                S.add("pe", lambda e, src=src, half=half, fc=fc, wrow=wrow, b0=b0: e.matmul(
                                    ps[b0 + half][:, :], lhsT=src[:, fc * 2048 + i * 128: fc * 2048 + (i + 1) * 128],
                                    rhs=wo[:, wrow * 1024 + half * 512: wrow * 1024 + (half + 1) * 512], start=(fc == 0), stop=(fc == 3)),
                                    reads=[sk, f"wo{wrow}"], writes=[f"ps{b0 + half}"])
                    T = t1[k]
                    for half in range(2):
                        hs = slice(half * 512, (half + 1) * 512)
                        S.add("dve", lambda e, half=half, hs=hs: e.scalar_tensor_tensor(out=T[:, hs], in0=ps[half][:, :], scalar=rstd_a[:, i:i + 1], in1=xres[k][:, hs],
                                                                                    op0=ALU.mult, op1=ALU.add),
                              reads=[f"ps{half}", "rstd_a", "xres0"], writes=[f"t1{k}"])
                        S.add("dve", lambda e, half=half, hs=hs: e.scalar_tensor_tensor(out=T[:, hs], in0=ps[2 + half][:, :], scalar=rstd_r[:, i:i + 1], in1=T[:, hs],
                                                                                    op0=ALU.mult, op1=ALU.add),
                              reads=[f"ps{2 + half}", "rstd_r", f"t1{k}"], writes=[f"t1{k}"])
                    for half in range(2):
                        S.add("dve", lambda e, half=half: e.bn_stats(out=stats[:, half * 6:(half + 1) * 6], in_=T[:, half * 512:(half + 1) * 512]),
                              reads=[f"t1{k}", "stats"], writes=["stats"])
                    S.add("dve", lambda e: e.bn_aggr(out=mv[:], in_=stats[:]), reads=["stats"], writes=["mv"])
                    S.add("dve", lambda e: e.tensor_scalar(out=rstd[:], in0=mv[:, 1:2], scalar1=LN_EPS, scalar2=None, op0=ALU.add), reads=["mv"], writes=["rstd"])
                    S.add("act", lambda e: e.activation(out=rstd[:], in_=rstd[:], func=AF.Ln), reads=["rstd"], writes=["rstd"])
                    S.add("act", lambda e: e.activation(out=rstd[:], in_=rstd[:], func=AF.Exp, scale=-0.5), reads=["rstd"], writes=["rstd"])
                    S.add("dve", lambda e: e.scalar_tensor_tensor(out=T[:], in0=T[:], scalar=mv[:, 0:1], in1=lng[:], op0=ALU.subtract, op1=ALU.mult),
                          reads=[f"t1{k}", "mv", "lng"], writes=[f"t1{k}"])
                    X1 = x1[:, i * 1024:(i + 1) * 1024]
                    S.add("dve", lambda e: e.scalar_tensor_tensor(out=X1, in0=T[:], scalar=rstd[:, 0:1], in1=lnb[:], op0=ALU.mult, op1=ALU.add),
                          reads=[f"t1{k}", "rstd", "lnb"], writes=[f"x1_{i}"])

                def do_tile_2c_B(i):
                    k = i % 2
                    X1 = x1[:, i * 1024:(i + 1) * 1024]
                    S.add("act", lambda e: e.copy(out=x1b[k][:], in_=X1), reads=[f"x1_{i}"], writes=[f"x1b{k}"])
                    pb = psb[4]
                    for dc in range(8):
                        S.add("pe", lambda e, dc=dc: e.transpose(out=pb[:, dc * 128:(dc + 1) * 128], in_=x1b[k][:, dc * 128:(dc + 1) * 128], identity=ident[:]),
                              reads=[f"x1b{k}", "ident"], writes=["ps4"])
                    dst = x1T[:].rearrange("p (c n) -> p c n", c=8)[:, :, i * 128:(i + 1) * 128]
                    srcp = pb[:].rearrange("p (c n) -> p c n", c=8)
                    S.add("act", lambda e: e.copy(out=dst, in_=srcp), reads=["ps4"], writes=["x1T"])

                wg = sb("wg", [128, 8 * 1024], BF16)
                wp = sb("wp", [128, 2 * 1024], BF16)
                wr = sb("wr", [128, 8 * 20], BF16)
                wrs = sb("wrs", [128, 8 * 20], F32)
                wst3 = wst
                rbias = sb("rbias", [128, 20], F32)
                pst = [sb(f"pst{k}", [128, 256], F32) for k in range(2)]
                pbb = [sb(f"pbb{k}", [128, 256], BF16) for k in range(2)]
                pT = [sb(f"pT{k}", [128, 256], BF16) for k in range(2)]
                sg = [sb("sg0", [128, 1024], F32)] * 2
                lg = sb("lg", [128, 20], F32)
                sm = sb("sm", [128, 16], F32)
                eg = sb("eg", [128, 4], F32)
                ohg = sb("ohg", [128, 4], F32)
                lsel = sb("lsel", [128, 8], F32)
                m8 = sb("m8", [128, 8], F32)
                wsel = sb("wsel", [128, 4], F32)
                wsel2 = sb("wsel2", [128, 4], F32)
                wg_keys = [f"wg{dc}" for dc in range(8)]

                def do_tile_3a(i):
                    k = i % 2
                    for dc in range(8):
                        S.add("pe", lambda e, dc=dc: e.matmul(ps[5][:, 0:20], lhsT=x1T[:, dc * 2048 + i * 128: dc * 2048 + (i + 1) * 128], rhs=wr[:, dc * 20:(dc + 1) * 20],
                                                               start=(dc == 0), stop=(dc == 7)),
                              reads=["x1T", "wr"], writes=["ps5"])
                    S.add("dve", lambda e: e.tensor_tensor(out=lg[:], in0=ps[5][:, 0:20], in1=rbias[:], op=ALU.add), reads=["ps5", "rbias"], writes=["lg"])
                    S.add("dve", lambda e: e.tensor_reduce(out=sm[:, 0:1], in_=lg[:, 0:4], axis=mybir.AxisListType.X, op=ALU.max), reads=["lg", "sm"], writes=["sm"])
                    S.add("dve", lambda e: e.tensor_scalar(out=sm[:, 1:2], in0=sm[:, 0:1], scalar1=-1.0, scalar2=None, op0=ALU.mult), reads=["sm"], writes=["sm"])
                    S.add("act", lambda e: e.activation(out=eg[:], in_=lg[:, 0:4], func=AF.Exp, bias=sm[:, 1:2], scale=1.0, accum_out=sm[:, 2:3]), reads=["lg", "sm"], writes=["eg", "sm"])
                    S.add("dve", lambda e: e.tensor_scalar(out=ohg[:], in0=lg[:, 0:4], scalar1=sm[:, 0:1], scalar2=None, op0=ALU.is_equal), reads=["lg", "sm"], writes=["ohg"])
                    S.add("dve", lambda e: e.tensor_scalar(out=lsel[:, 0:4], in0=lg[:, 4:8], scalar1=ohg[:, 0:1], scalar2=None, op0=ALU.mult), reads=["lg", "ohg", "lsel"], writes=["lsel"])
                    for g in range(1, 4):
                        S.add("dve", lambda e, g=g: e.scalar_tensor_tensor(out=lsel[:, 0:4], in0=lg[:, 4 + 4 * g:8 + 4 * g], scalar=ohg[:, g:g + 1], in1=lsel[:, 0:4],
                                                                          op0=ALU.mult, op1=ALU.add),
                              reads=["lg", "ohg", "lsel"], writes=["lsel"])
                    S.add("dve", lambda e: e.max(out=m8[:], in_=lsel[:]), reads=["lsel"], writes=["m8"])
                    S.add("dve", lambda e: e.tensor_tensor(out=sm[:, 4:5], in0=m8[:, 1:2], in1=m8[:, 0:1], op=ALU.subtract), reads=["m8", "sm"], writes=["sm"])
                    S.add("act", lambda e: e.activation(out=sm[:, 5:6], in_=sm[:, 4:5], func=AF.Exp), reads=["sm"], writes=["sm"])
                    S.add("dve", lambda e: e.tensor_scalar(out=sm[:, 6:7], in0=sm[:, 5:6], scalar1=1.0, scalar2=sm[:, 2:3], op0=ALU.add, op1=ALU.mult), reads=["sm"], writes=["sm"])
                    S.add("dve", lambda e: e.reciprocal(out=sm[:, 7:8], in_=sm[:, 6:7]), reads=["sm"], writes=["sm"])
                    S.add("dve", lambda e: e.tensor_tensor(out=sm[:, 8:9], in0=sm[:, 7:8], in1=sm[:, 5:6], op=ALU.mult), reads=["sm"], writes=["sm"])
                    S.add("dve", lambda e: e.tensor_scalar(out=wsel[:], in0=lsel[:, 0:4], scalar1=m8[:, 0:1], scalar2=sm[:, 7:8], op0=ALU.is_equal, op1=ALU.mult),
                          reads=["lsel", "m8", "sm"], writes=["wsel"])
                    S.add("dve", lambda e: e.tensor_scalar(out=wsel2[:], in0=lsel[:, 0:4], scalar1=m8[:, 1:2], scalar2=sm[:, 8:9], op0=ALU.is_equal, op1=ALU.mult),
                          reads=["lsel", "m8", "sm"], writes=["wsel2"])
                    S.add("dve", lambda e: e.tensor_tensor(out=wsel[:], in0=wsel[:], in1=wsel2[:], op=ALU.add), reads=["wsel", "wsel2"], writes=["wsel"])
                    cdst = comb[:, i * 16:(i + 1) * 16].rearrange("p (g e) -> p g e", g=4)
                    S.add("dve", lambda e: e.tensor_tensor(out=cdst, in0=ohg[:, 0:4].unsqueeze(2).to_broadcast([128, 4, 4]),
                                                           in1=wsel[:, 0:4].unsqueeze(1).to_broadcast([128, 4, 4]), op=ALU.mult),
                          reads=["wsel", "ohg"], writes=["comb"])
                    S.add("sp", lambda e: e.dma_start(out=pst[k][:], in_=po[i * 128:(i + 1) * 128, :]), writes=[f"pst{k}"], dma=True)
                    S.add("pool", lambda e: e.tensor_copy(out=pbb[k][:], in_=pst[k][:]), reads=[f"pst{k}"], writes=[f"pbb{k}"])
                    for kc in range(2):
                        S.add("pe", lambda e, kc=kc: e.transpose(out=psb[5][:, 512 + kc * 128:512 + (kc + 1) * 128], in_=pbb[k][:, kc * 128:(kc + 1) * 128], identity=ident[:]),
                              reads=[f"pbb{k}", "ident"], writes=["ps5"])
                    S.add("dve", lambda e: e.tensor_copy(out=pT[k][:], in_=psb[5][:, 512:768]), reads=["ps5"], writes=[f"pT{k}"])
                    G_ = sg[k]
                    for half in range(2):
                        for dc in range(8):
                            S.add("pe", lambda e, half=half, dc=dc: e.matmul(ps[6 + half][:, :], lhsT=x1T[:, dc * 2048 + i * 128: dc * 2048 + (i + 1) * 128],
                                                                           rhs=wg[:, dc * 1024 + half * 512: dc * 1024 + (half + 1) * 512], start=(dc == 0), stop=(dc == 7)),
                                  reads=["x1T", f"wg{dc}"], writes=[f"ps{6 + half}"])
                    for half in range(2):
                        hs = slice(half * 512, (half + 1) * 512)
                        S.add("act", lambda e, half=half, hs=hs: e.activation(out=G_[:, hs], in_=ps[6 + half][:, :], func=AF.Tanh, scale=0.5), reads=[f"ps{6 + half}"], writes=["sg0"])
                    for half in range(2):
                        for kc in range(2):
                            S.add("pe", lambda e, half=half, kc=kc: e.matmul(ps[6 + half][:, :], lhsT=pT[k][:, kc * 128:(kc + 1) * 128],
                                                                           rhs=wp[:, kc * 1024 + half * 512: kc * 1024 + (half + 1) * 512], start=(kc == 0), stop=(kc == 1)),
                                  reads=[f"pT{k}", f"wp{kc}"], writes=[f"ps{6 + half}"])
                    for half in range(2):
                        hs = slice(half * 512, (half + 1) * 512)
                        S.add("dve", lambda e, half=half, hs=hs: e.scalar_tensor_tensor(out=G_[:, hs], in0=G_[:, hs], scalar=1.0, in1=ps[6 + half][:, :], op0=ALU.add, op1=ALU.mult),
                              reads=["sg0", f"ps{6 + half}"], writes=["sg0"])
                    X1 = x1[:, i * 1024:(i + 1) * 1024]
                    S.add("dve", lambda e: e.scalar_tensor_tensor(out=X1, in0=X1, scalar=ALPHA, in1=G_[:], op0=ALU.mult, op1=ALU.add), reads=["sg0", f"x1_{i}"], writes=[f"x1_{i}"])

                do_tile_2c(0)
                S.add("pool", lambda e: e.memset(lsel[:], -1.0e30), writes=["lsel"])
                S.add("sp", lambda e: e.dma_start(out=rbias[:], in_=rb_d[0:1, :].partition_broadcast(128)), writes=["rbias"], dma=True)
                S.add("sp", lambda e: e.dma_start(out=wrs[:].rearrange("p (c n) -> p c n", c=8), in_=wr_d[:, :].rearrange("(c p) n -> p c n", p=128)), writes=["wrs"], dma=True)
                S.add("dve", lambda e: e.tensor_copy(out=wr[:], in_=wrs[:]), reads=["wrs"], writes=["wr"])
                for dc in range(8):
                    S.add("sp", lambda e, dc=dc: e.dma_start(out=wst3[:], in_=wg_d[dc * 128:(dc + 1) * 128, :]), writes=["wst"], dma=True)
                    S.add("dve", lambda e, dc=dc: e.tensor_copy(out=wg[:, dc * 1024:(dc + 1) * 1024], in_=wst3[:]), reads=["wst"], writes=[f"wg{dc}"])
                for kc in range(2):
                    S.add("sp", lambda e, kc=kc: e.dma_start(out=wst3[:], in_=wp_d[kc * 128:(kc + 1) * 128, :]), writes=["wst"], dma=True)
                    S.add("dve", lambda e, kc=kc: e.tensor_scalar(out=wp[:, kc * 1024:(kc + 1) * 1024], in0=wst3[:], scalar1=0.5, scalar2=None, op0=ALU.mult), reads=["wst"], writes=[f"wp{kc}"])
                for i in range(NOWN):
                    if i + 1 < NOWN:
                        do_tile_2c(i + 1)
                    if i >= 1:
                        do_tile_3a(i - 1)
                    do_tile_2c_B(i)
                do_tile_3a(NOWN - 1)
                if "p2c" in dbg:
                    es.dump_tmp = wst
                    dump(S, es, "x1", x1, F32)
                S.drain()
                with nc.Block() as block:
                    base = S.emit(block, sems)
            r_scope.close()
            if "stop2c" in dbg:
                return nc

            with ExitStack() as es:
                def sb(name, shape, dt):
                    return es.enter_context(nc.sbuf_tensor("p3b_" + name, list(shape), dt))
                S = Sched(base)
                wu = [sb(f"wu{k}", [128, 8 * 1024], BF16) for k in range(2)]
                wd = [sb(f"wd{k}", [128, 4 * 1024], BF16) for k in range(2)]
                NST = 3
                wst = [sb(f"wst{k}", [128, 1024], F32) for k in range(NST)]
                sa = [sb(f"sa{k}", [128, 512], F32) for k in range(2)]
                H = [[sb(f"H{p}_{fc}", [128, 512], BF16) for fc in range(4)] for p in range(2)]
                lng = sb("lng", [128, 1024], F32)
                lnb = sb("lnb", [128, 1024], F32)
                stats = sb("stats", [128, 12], F32)
                mv = sb("mv", [128, 2], F32)
                rstd = sb("rstd", [128, 1], F32)
                yb = [sb(f"yb{k}", [128, 1024], F32) for k in range(2)]
                S.add("sp", lambda e: e.dma_start(out=lng[:], in_=lnp_d[2:3, :].partition_broadcast(128)), writes=["lng"], dma=True)
                S.add("sp", lambda e: e.dma_start(out=lnb[:], in_=lnp_d[3:4, :].partition_broadcast(128)), writes=["lnb"], dma=True)
                stc = [0]

                def load_expert(e_):
                    p = e_ % 2
                    for kind, nchunk, srcd, dstt in (("u", 8, wup_d, wu[p]), ("d", 4, wdn_d, wd[p])):
                        for c in range(nchunk):
                            sidx = stc[0] % NST
                            stc[0] += 1
                            st = wst[sidx]
                            S.add("sp", lambda e, srcd=srcd, c=c, st=st: e.dma_start(out=st[:], in_=srcd[e_, c * 128:(c + 1) * 128, :]), writes=[f"wst{sidx}"], dma=True)
                            key = f"w{kind}{p}_{c}"
                            S.add("pool", lambda e, dstt=dstt, c=c, st=st: e.tensor_copy(out=dstt[:, c * 1024:(c + 1) * 1024], in_=st[:]), reads=[f"wst{sidx}"], writes=[key])

                actr = [0]

                def up_chunk(e_, tc):
                    p = e_ % 2
                    hp = (e_ * 4 + tc) % 2
                    wkeys = [f"wu{p}_{c}" for c in range(8)]
                    for fc in range(4):
                        pa = (fc % 2) * 2
                        for ab in range(2):
                            m = fc + 4 * ab
                            for dc in range(8):
                                S.add("pe", lambda e, dc=dc, m=m, pa=pa, ab=ab: e.matmul(ps[pa + ab][:, :], lhsT=wu[p][:, dc * 1024 + m * 128: dc * 1024 + (m + 1) * 128],
                                                                                     rhs=x1T[:, dc * 2048 + tc * 512: dc * 2048 + (tc + 1) * 512], start=(dc == 0), stop=(dc == 7)),
                                      reads=["x1T", f"wu{p}_{dc}"], writes=[f"ps{pa + ab}"])
                        sk = actr[0] % 2
                        actr[0] += 1
                        S.add("act", lambda e, pa=pa, sk=sk: e.activation(out=sa[sk][:], in_=ps[pa][:, :], func=AF.Silu), reads=[f"ps{pa}"], writes=[f"sa{sk}"])
                        S.add("dve", lambda e, pa=pa, sk=sk, fc=fc: e.tensor_tensor(out=H[hp][fc][:], in0=sa[sk][:], in1=ps[pa + 1][:, :], op=ALU.mult),
                              reads=[f"sa{sk}", f"ps{pa + 1}"], writes=[f"H{hp}_{fc}"])

                dctr = [0]

                def down_chunk(e_, tc):
                    p = e_ % 2
                    hp = (e_ * 4 + tc) % 2
                    wkeys = [f"wd{p}_{c}" for c in range(4)]
                    for tt in range(4):
                        i = tc * 4 + tt
                        for half in range(2):
                            bank = 4 + dctr[0] % 4
                            dctr[0] += 1
                            for fc in range(4):
                                S.add("pe", lambda e, fc=fc, tt=tt, half=half, bank=bank: e.matmul(ps[bank][:, :], lhsT=H[hp][fc][:, tt * 128:(tt + 1) * 128],
                                                                                               rhs=wd[p][:, fc * 1024 + half * 512: fc * 1024 + (half + 1) * 512],
                                                                                               start=(fc == 0), stop=(fc == 3)),
                                      reads=[f"H{hp}_{fc}", f"wd{p}_{fc}"], writes=[f"ps{bank}"])
                            A_ = x1[:, i * 1024 + half * 512: i * 1024 + (half + 1) * 512]
                            S.add("dve", lambda e, bank=bank, A_=A_, i=i: e.scalar_tensor_tensor(out=A_, in0=ps[bank][:, :], scalar=comb[:, i * 16 + e_: i * 16 + e_ + 1], in1=A_,
                                                                                               op0=ALU.mult, op1=ALU.add),
                                  reads=[f"ps{bank}", "comb", f"x1_{i}"], writes=[f"x1_{i}"])
                        if e_ == 15:
                            ln2_store(i)

                def ln2_store(i):
                    k = i % 2
                    T = x1[:, i * 1024:(i + 1) * 1024]
                    Y = yb[k]
                    for half in range(2):
                        S.add("dve", lambda e, half=half: e.bn_stats(out=stats[:, half * 6:(half + 1) * 6], in_=T[:, half * 512:(half + 1) * 512]),
                              reads=[f"x1_{i}", "stats"], writes=["stats"])
                    S.add("dve", lambda e: e.bn_aggr(out=mv[:], in_=stats[:]), reads=["stats"], writes=["mv"])
                    S.add("dve", lambda e: e.tensor_scalar(out=rstd[:], in0=mv[:, 1:2], scalar1=LN_EPS, scalar2=None, op0=ALU.add), reads=["mv"], writes=["rstd"])
                    S.add("act", lambda e: e.activation(out=rstd[:], in_=rstd[:], func=AF.Ln), reads=["rstd"], writes=["rstd"])
                    S.add("act", lambda e: e.activation(out=rstd[:], in_=rstd[:], func=AF.Exp, scale=-0.5), reads=["rstd"], writes=["rstd"])
                    S.add("dve", lambda e: e.scalar_tensor_tensor(out=Y[:], in0=T, scalar=mv[:, 0:1], in1=lng[:], op0=ALU.subtract, op1=ALU.mult),
                          reads=[f"x1_{i}", "mv", "lng"], writes=[f"yb{k}"])
                    S.add("dve", lambda e: e.scalar_tensor_tensor(out=Y[:], in0=Y[:], scalar=rstd[:, 0:1], in1=lnb[:], op0=ALU.mult, op1=ALU.add),
                          reads=[f"yb{k}", "rstd", "lnb"], writes=[f"yb{k}"])
                    S.add("sp", lambda e: e.dma_start(out=out_d[i * 128:(i + 1) * 128, :], in_=Y[:]), reads=[f"yb{k}"], writes=[f"out{i}"], dma=True)

                load_expert(0)
                seq = [(e_, tc) for e_ in range(16) for tc in range(4)]
                prev = None
                for (e_, tc) in seq:
                    up_chunk(e_, tc)
                    if prev is not None:
                        down_chunk(*prev)
                    if tc == 0 and e_ + 1 < 16:
                        load_expert(e_ + 1)
                    prev = (e_, tc)
                down_chunk(*prev)
                S.drain()
                with nc.Block() as block:
                    base = S.emit(block, sems)
    return nc


def host_prep(inputs):
    f = lambda k: np.asarray(inputs[k], dtype=np.float32)
    x = f("x")
    p = f("p")[0]
    w_in = f("w_in")[0]
    rel_bias = f("rel_bias")
    q_cols = np.arange(0, 512)
    kv0 = 512
    kc, vc, ks, vs, kw, vw = [np.arange(kv0 + 128 * k, kv0 + 128 * (k + 1)) for k in range(6)]
    g_cols = np.arange(1280, 1304)
    rx = np.arange(1304, 1816)
    ry = np.arange(1816, 2328)
    colsA = np.concatenate([kc, vc, ks, kw, rx, vs, vw])
    qperm = np.concatenate([np.concatenate([np.arange(64 * r, 64 * r + 64), np.arange(64 * (4 + r), 64 * (4 + r) + 64)]) for r in range(4)])
    colsC = np.concatenate([q_cols[qperm], ry, g_cols])
    rep = {}
    rep["wA"] = np.ascontiguousarray(w_in[:, colsA])
    rep["wC"] = np.ascontiguousarray(w_in[:, colsC])
    ki = np.arange(128)[:, None]
    qi = np.arange(128)[None, :]
    tabs = np.zeros((NTAB, 128, 8, 128), np.float32)

    def fill(tid, d, valid):
        bk = _bucket_exact(d)
        for h in range(8):
            v = rel_bias[bk, h]
            tabs[tid, :, h, :] = np.where(valid, v, np.float32(MASKV))
    fill(T_D, qi - ki, (qi - ki) >= 0)
    fill(T_O1, 128 + qi - ki, np.ones((128, 128), bool))
    fill(T_W4, 512 + qi - ki, (512 + qi - ki) < 512)
    fill(T_C, np.full((128, 128), 1000), np.ones((128, 128), bool))
    for m in range(4):
        d = 512 * m + 369 + qi - 16 * ki
        fill(T_N0 + m, d, d >= 0)
    rep["tabs"] = tabs.reshape(NTAB, 128, 1024)
    rep["cb"] = np.ascontiguousarray(np.broadcast_to(rel_bias[31, :][None, :], (128, 8))).astype(np.float32)
    s_ix = np.arange(128)[:, None]
    c_ix = np.arange(SEQ)[None, :]
    rep["E"] = (c_ix // 64 == s_ix).astype(np.float32)
    conv_w = f("conv_w")[0]
    rep["convw"] = np.ascontiguousarray(conv_w.reshape(4, 4, 128).transpose(2, 0, 1).reshape(128, 16))
    vec = lambda k: f(k)[0].reshape(4, 128).T
    rep["rgvec"] = np.ascontiguousarray(np.concatenate([vec("conv_b"), vec("rg_b_a"), vec("rg_b_x"), vec("rg_lambda")], axis=1))

    def bd(w):
        o = np.zeros((128, 4, 128), np.float32)
        for ct in range(4):
            for e in range(2):
                o[e * 64:(e + 1) * 64, ct, e * 64:(e + 1) * 64] = w[2 * ct + e]
        return o.reshape(128, 512)
    rep["BDa"] = bd(f("rg_w_a")[0])
    rep["BDx"] = bd(f("rg_w_x")[0])

    def bdw(w):
        o = np.zeros((128, 32, 128), np.float32)
        wl = w.reshape(32, 64, 64)
        for g in range(2):
            o[g * 64:(g + 1) * 64, :, g * 64:(g + 1) * 64] = wl.transpose(1, 0, 2)
        return o.reshape(128, 32 * 128)
    rep["BDWk"] = bdw(f("cmp_w_k")[0])
    rep["BDWv"] = bdw(f("cmp_w_v")[0])
    pk = f("cmp_pe_k")[0].T
    pv = f("cmp_pe_v")[0].T
    rep["pe2"] = np.ascontiguousarray(np.concatenate([np.concatenate([pk, pk], 0), np.concatenate([pv, pv], 0)], axis=1))
    cp = np.arange(512)[:, None] - 1
    sl = np.arange(128)[None, :]
    ovm = ((16 * cp < 64 * sl + 64) & (16 * cp + 32 > 64 * sl)).astype(np.float32)
    rep["ov"] = np.ascontiguousarray(ovm.reshape(4, 128, 128).transpose(1, 0, 2).reshape(128, 512))
    gain = np.concatenate([f("attn_out_gain")[0], f("rnn_out_gain")[0]])
    rep["gain"] = np.ascontiguousarray(gain.reshape(8, 128).T)
    rep["wout"] = f("w_out")[0]
    rep["lnp"] = np.stack([f("ln1_g")[0], f("ln1_b")[0], f("ln2_g")[0], f("ln2_b")[0]])
    rep["wr"] = np.ascontiguousarray(np.concatenate([f("router_group_w")[0], f("router_expert_w")[0]], axis=1))
    rep["rb"] = np.concatenate([f("router_group_b")[0], f("router_expert_b")[0]])[None, :]
    rep["wup"] = f("expert_w_up")[0]
    rep["wdn"] = f("expert_w_down")[0]
    rep["wp"] = f("ple_w")[0]
    rep["wg"] = f("ple_gate_w")[0]
    in_maps = []
    for c in range(8):
        b, j = divmod(c, 4)
        sh = (3 - j) * 128
        m = dict(rep)
        xs_ = np.zeros((SEQ, D_MODEL), np.float32)
        xs_[sh:] = x[b, :SEQ - sh]
        m["xs"] = xs_
        own = np.concatenate([np.arange((4 * i + j) * 128, (4 * i + j + 1) * 128) for i in range(NOWN)])
        m["po"] = np.ascontiguousarray(p[b, own])
        kb = np.zeros((128, NT), np.float32)
        kb[:, :3 - j] = MASKV
        m["keybias"] = kb
        cpi = np.arange(512) - 1
        cbv = np.where(cpi >= 8 * (3 - j), 0.0, MASKV).astype(np.float32)
        m["cbias"] = np.ascontiguousarray(cbv.reshape(4, 128).T)
        sbias = np.zeros((NOWN, 128, 128), np.float32)
        s0 = 2 * (3 - j)
        for i in range(NOWN):
            qpos = 128 * (4 * i + 3) + np.arange(128)[:, None]
            cur = qpos // 64
            sblk = np.arange(128)[None, :]
            valid = (sblk <= cur) & (sblk >= s0)
            forced = (sblk == s0) | (sblk == cur) | (sblk == cur - 1)
            sbias[i] = np.where(valid, np.where(forced, 1e4, 0.0), -1e30)
        m["selbias"] = sbias
        pm = np.ones((128, 512), np.float32)
        pm[:, :sh] = 0.0
        m["padmask"] = pm
        in_maps.append(m)
    return in_maps


def kernel(**inputs):
    in_maps = host_prep(inputs)
    nc = build_program()
    res = run_bass_kernel_spmd(nc, in_maps, core_ids=list(range(8)))
    out = np.zeros((2, SEQ, D_MODEL), np.float32)
    for c in range(8):
        b, j = divmod(c, 4)
        o = res.results[c]["out"]
        for i in range(NOWN):
            n = 4 * i + j
            out[b, n * 128:(n + 1) * 128] = o[i * 128:(i + 1) * 128]
    return out
```

```python
import numpy as np
from contextlib import ExitStack
import concourse.bass as bass
import concourse.mybir as mybir
from concourse.bass_utils import run_bass_kernel_spmd

F32 = mybir.dt.float32
BF16 = mybir.dt.bfloat16
AF = mybir.ActivationFunctionType
ALU = mybir.AluOpType

COMPUTE = ("pe", "act", "dve", "pool")
STREAMS = ("pe", "act", "dve", "pool", "sp")
NRING = 8
ENGMAP = {"pe": "tensor", "act": "scalar", "dve": "vector", "pool": "gpsimd", "sp": "sync"}


class Op:
    __slots__ = ("eng", "fn", "dma", "deps", "signal", "pos", "ring", "ringval", "idx", "waits")

    def __init__(self, eng, fn, dma):
        self.eng = eng
        self.fn = fn
        self.dma = dma
        self.deps = []
        self.signal = False
        self.pos = None
        self.ring = None
        self.ringval = None
        self.waits = None


class Sched:
    def __init__(self, base):
        self.base = base
        self.streams = {e: [] for e in STREAMS}
        self.last_writer = {}
        self.readers = {}
        self.dma_count = {s: 0 for s in STREAMS}
        self.dma_hist = {s: [] for s in STREAMS}

    def add(self, eng, fn, reads=(), writes=(), dma=False):
        op = Op(eng, fn, dma)
        op.idx = len(self.streams[eng])
        deps = []
        for b in reads:
            w = self.last_writer.get(b)
            if w is not None:
                deps.append(w)
        for b in writes:
            w = self.last_writer.get(b)
            if w is not None:
                deps.append(w)
            deps.extend(self.readers.get(b, ()))
        if dma:
            k = self.dma_count[eng]
            self.dma_count[eng] += 1
            op.ring = k % NRING
            op.ringval = 16 * (k // NRING + 1)
            hist = self.dma_hist[eng]
            if k >= NRING:
                deps.append(hist[k - NRING])
            hist.append(op)
        best = {}
        for d in deps:
            if d is op:
                continue
            if d.dma:
                key = ("dma", d.eng, d.ring)
                cur = best.get(key)
                if cur is None or d.ringval > cur.ringval:
                    best[key] = d
            else:
                if d.eng == op.eng and not op.dma and op.eng == "pe":
                    continue
                cur = best.get(d.eng)
                if cur is None or d.idx > cur.idx:
                    best[d.eng] = d
        for d in best.values():
            if not d.dma:
                d.signal = True
            op.deps.append(d)
        self.streams[eng].append(op)
        for b in writes:
            self.last_writer[b] = op
            self.readers[b] = []
        for b in reads:
            self.readers.setdefault(b, []).append(op)
        return op

    def drain(self):
        op = Op("sp", None, False)
        op.idx = len(self.streams["sp"])
        for s in STREAMS:
            hist = self.dma_hist[s]
            for r in range(NRING):
                last = None
                for o in hist:
                    if o.ring == r:
                        last = o
                if last is not None:
                    op.deps.append(last)
        self.streams["sp"].append(op)

    def emit(self, block, sems):
        base = self.base
        for e in COMPUTE:
            c = 0
            for op in self.streams[e]:
                if op.signal:
                    c += 1
                    op.pos = base.get(e, 0) + c
        for s in STREAMS:
            seen = {}
            for op in self.streams[s]:
                op.waits = []
                for d in op.deps:
                    if d.dma:
                        key = (d.eng, d.ring)
                        val = base.get(key, 0) + d.ringval
                    else:
                        key = d.eng
                        val = d.pos
                    if seen.get(key, 0) >= val:
                        continue
                    seen[key] = val
                    op.waits.append((key, val))

        def body_for(sname):
            ops = self.streams[sname]

            def body(eng):
                for op in ops:
                    for key, val in op.waits:
                        eng.wait_ge(sems[key], val)
                    if op.fn is None:
                        continue
                    inst = op.fn(eng)
                    if op.dma:
                        inst.then_inc(sems[(op.eng, op.ring)], 16)
                    elif op.signal:
                        inst.then_inc(sems[op.eng], 1)
            return body

        for sname in STREAMS:
            if self.streams[sname]:
                getattr(block, ENGMAP[sname])(body_for(sname))
        nb = dict(base)
        for e in COMPUTE:
            nb[e] = base.get(e, 0) + sum(1 for op in self.streams[e] if op.signal)
        for s in STREAMS:
            for r in range(NRING):
                n = sum(1 for o in self.dma_hist[s] if o.ring == r)
                if n:
                    nb[(s, r)] = base.get((s, r), 0) + 16 * n
        return nb


D_MODEL = 1024
SEQ = 8192
NT = 64
NOWN = 16
ALPHA = 2.0 ** 0.25
LN_EPS = 1e-5
RMS_EPS = 1e-6
MASKV = -256.0
N_BUCKETS = 32
T_D, T_O1, T_W4, T_C, T_N0 = 0, 1, 2, 3, 4
NTAB = 8


def t5_bucket_np(d):
    d = np.maximum(d, 0)
    df = np.maximum(d, 1).astype(np.float32)
    large = 16 + (np.log(df / np.float32(16)) / np.float32(np.log(128 / 16)) * np.float32(16)).astype(np.int32)
    large = np.minimum(large, 31)
    return np.where(d < 16, d, large)


def _bucket_exact(d):
    import math
    d = np.maximum(d, 0)
    df = np.maximum(d, 1).astype(np.float32)
    v = np.log(df / np.float32(16.0)).astype(np.float32) / np.float32(math.log(128 / 16))
    large = 16 + (v * np.float32(16)).astype(np.int32)
    large = np.minimum(large, 31)
    return np.where(d < 16, d, large)


def build_program(dbg=()):
    nc = bass.Bass("TRN2", target_bir_lowering=False)

    def din(name, shape, dt=F32):
        return nc.dram_tensor(name, list(shape), dt, kind="ExternalInput").ap()

    def dout(name, shape, dt=F32):
        return nc.dram_tensor(name, list(shape), dt, kind="ExternalOutput").ap()

    _shapes = dict(
        xs=[SEQ, D_MODEL], po=[NOWN * 128, 256], keybias=[128, NT], cbias=[128, 4], selbias=[NOWN, 128, 128],
        padmask=[128, 512], wA=[D_MODEL, 1280], wC=[D_MODEL, 1048], tabs=[NTAB, 128, 1024], cb=[128, 8],
        E=[128, SEQ], convw=[128, 16], rgvec=[128, 16], BDa=[128, 512], BDx=[128, 512], BDWk=[128, 32 * 128],
        BDWv=[128, 32 * 128], pe2=[128, 64], ov=[128, 4 * 128], gain=[128, 8], wout=[D_MODEL, D_MODEL],
        lnp=[4, D_MODEL], wr=[D_MODEL, 20], rb=[1, 20], wup=[16, D_MODEL, 1024], wdn=[16, 512, D_MODEL],
        wp=[256, D_MODEL], wg=[D_MODEL, D_MODEL])
    _decl = {}

    class _Lazy:
        def __init__(self, name):
            self.name = name

        def _ap(self):
            if self.name not in _decl:
                _decl[self.name] = din(self.name, _shapes[self.name])
            return _decl[self.name]

        def __getitem__(self, key):
            return self._ap()[key]

        @property
        def tensor(self):
            return self._ap().tensor
    nc._used_inputs = _decl
    xs, po, keybias_d, cbias_d, selbias_d, padmask_d = [_Lazy(n) for n in ("xs", "po", "keybias", "cbias", "selbias", "padmask")]
    wA_d, wC_d, tabs_d, cb_d, E_d, convw_d, rgvec_d, BDa_d, BDx_d, BDWk_d, BDWv_d, pe2_d, ov_d, gain_d = [
        _Lazy(n) for n in ("wA", "wC", "tabs", "cb", "E", "convw", "rgvec", "BDa", "BDx", "BDWk", "BDWv", "pe2", "ov", "gain")]
    wout_d, lnp_d, wr_d, rb_d, wup_d, wdn_d, wp_d, wg_d = [_Lazy(n) for n in ("wout", "lnp", "wr", "rb", "wup", "wdn", "wp", "wg")]
    out_d = None if "stop1" in dbg or "stop2a" in dbg or "stop2b" in dbg or "stop2c" in dbg else dout("out", [NOWN * 128, D_MODEL])
    dbg_t = {}
    if "p1" in dbg:
        dbg_t["KsT"] = dout("d_KsT", [128, SEQ])
        dbg_t["KwT"] = dout("d_KwT", [128, SEQ])
        dbg_t["Vs"] = dout("d_Vs", [128, NT * 130])
        dbg_t["KcT"] = dout("d_KcT", [128, 512])
        dbg_t["OVV"] = dout("d_OVV", [128, 4 * 2 * 193])
        dbg_t["hown"] = dout("d_hown", [128, 4 * 2048])
    if "p2a" in dbg:
        dbg_t["QT"] = dout("d_QT", [128, NOWN * 512])
        dbg_t["rnnT"] = dout("d_rnnT", [128, 4 * 2048])
        dbg_t["gates"] = dout("d_gates", [128, NOWN * 24])
        dbg_t["ssr"] = dout("d_ssr", [128, NOWN])
    if "p2b" in dbg:
        dbg_t["attnT"] = dout("d_attnT", [128, 4 * 2048])
        dbg_t["rstda"] = dout("d_rstda", [128, NOWN])
        dbg_t["sel"] = dout("d_sel", [128, NOWN * 2 * 128])
    if "p2c" in dbg:
        dbg_t["x1"] = dout("d_x1", [128, NOWN * 1024])

    with ExitStack() as top:
        def tsb(name, shape, dt):
            return top.enter_context(nc.sbuf_tensor("sb_" + name, list(shape), dt))

        sems = {}
        for k in list(COMPUTE) + [("sp", r) for r in range(NRING)] + [("pool", r) for r in range(NRING)]:
            nm = k if isinstance(k, str) else f"{k[0]}{k[1]}"
            sems[k] = top.enter_context(nc.semaphore("s_" + nm))
        ps = [top.enter_context(nc.psum_tensor(f"ps{k}", [128, 512], F32)) for k in range(8)]
        psb = [p[:].bitcast(BF16) for p in ps]

        ident = tsb("ident", [128, 128], BF16)
        identf = tsb("identf", [128, 128], F32)
        base = {}
        ssr = tsb("ssr", [128, NOWN], F32)
        rstd_a = tsb("rstd_a", [128, NOWN], F32)

        def dump(S, es, name, src_ap, dt):
            d = dbg_t[name]
            n = d.shape[1]
            CH = 1024
            tmp = es.dump_tmp
            for c0 in range(0, n, CH):
                c1 = min(n, c0 + CH)
                S.add("pool", lambda e, c0=c0, c1=c1: e.tensor_copy(out=tmp[:, 0:c1 - c0], in_=src_ap[:, c0:c1]),
                      reads=[], writes=["dmp"])
                S.add("sp", lambda e, c0=c0, c1=c1: e.dma_start(out=d[:, c0:c1], in_=tmp[:, 0:c1 - c0]),
                      reads=["dmp"], writes=[f"dump_{name}_{c0}"], dma=True)

        with ExitStack() as attn_scope:
            def asb(name, shape, dt):
                return attn_scope.enter_context(nc.sbuf_tensor("sb_" + name, list(shape), dt))
            KsT = asb("KsT", [128, SEQ], BF16)
            KwT = asb("KwT", [128, SEQ], BF16)
            Vs = asb("Vs", [128, NT * 130 + 64], BF16)
            Vw = asb("Vw", [128, NT * 130 + 64], BF16)
            KcT = asb("KcT", [128, 512], BF16)
            OVV = asb("OVV", [128, 4 * 2 * 193], BF16)

            with ExitStack() as es:
                def sb(name, shape, dt):
                    return es.enter_context(nc.sbuf_tensor("sb_" + name, list(shape), dt))
                S = Sched(base)
                h_scope = ExitStack()
                hown = h_scope.enter_context(nc.sbuf_tensor("sb_hown", [128, 4 * 2048], BF16))
                wA = sb("wA", [128, 8 * 1280], BF16)
                wst = [sb("wst0", [128, 1280], F32)] * 2
                xst = [sb(f"xst{k}", [128, 1024], F32) for k in range(2)]
                xb = [sb(f"xb{k}", [128, 1024], BF16) for k in range(2)]
                xT = [sb(f"xT{k}", [128, 8 * 512], BF16) for k in range(2)]
                kraw = sb("kraw", [128, 16 + 2048 + 32], BF16)
                vraw = sb("vraw", [128, 16 + 2048 + 32], BF16)
                BDWk = sb("BDWk", [128, 32 * 128], BF16)
                BDWv = sb("BDWv", [128, 32 * 128], BF16)
                pe2 = sb("pe2", [128, 64], F32)
                pe2b = sb("pe2b", [128, 64], BF16)
                cbk = sb("cbk", [128, 2], F32)
                vcs = sb("vcs", [128, 128], BF16)
                convw = sb("convw", [128, 16], F32)
                rgvec = sb("rgvec", [128, 16], F32)
                rgc = sb("rgc", [128, 20], F32)
                dg = sb("dg", [128, 16 * 128], BF16)
                BDa = sb("BDa", [128, 512], BF16)
                BDx = sb("BDx", [128, 512], BF16)
                bdst = sb("bdst", [128, 512], F32)
                rxb = [[sb(f"rxb{p}_{ct}", [128, 515], BF16) for ct in range(4)] for p in range(2)]
                NR = 2
                xcf = [sb(f"xcf{k}", [128, 512], F32) for k in range(NR)]
                xcb = [sb(f"xcb{k}", [128, 512], BF16) for k in range(NR)]
                thr_ = [sb(f"thr{k}", [128, 512], F32) for k in range(NR)]
                thx = [sb(f"thx{k}", [128, 512], F32) for k in range(NR)]
                av = [sb(f"av{k}", [128, 512], F32) for k in range(NR)]
                hb = [sb(f"hb{k}", [128, 512], F32) for k in range(NR)]
                carry = sb("carry", [128, 4], F32)
                padmask = sb("padmask", [128, 512], F32)

                S.add("pool", lambda e: e.memset(identf[:], 0.0), writes=["identf"])
                S.add("pool", lambda e: e.affine_select(out=identf[:], in_=identf[:], compare_op=ALU.not_equal, fill=1.0,
                                                        base=0, pattern=[[-1, 128]], channel_multiplier=1),
                      reads=["identf"], writes=["identf"])
                S.add("pool", lambda e: e.tensor_copy(out=ident[:], in_=identf[:]), reads=["identf"], writes=["ident"])
                S.add("pool", lambda e: e.memset(carry[:], 0.0), writes=["carry"])
                S.add("pool", lambda e: e.memset(kraw[:], 0.0), writes=["kraw"])
                S.add("pool", lambda e: e.memset(vraw[:], 0.0), writes=["vraw"])
                S.add("pool", lambda e: e.memset(Vs[:], 1.0), writes=["Vs"])
                S.add("pool", lambda e: e.memset(Vw[:], 1.0), writes=["Vw"])
                S.add("pool", lambda e: e.memset(OVV[:], 1.0), writes=["OVV"])
                for p in range(2):
                    for ct in range(4):
                        S.add("pool", lambda e, p=p, ct=ct: e.memset(rxb[p][ct][:, 0:3], 0.0), writes=[f"rxb{p}_{ct}"])
                S.add("sp", lambda e: e.dma_start(out=convw[:], in_=convw_d[:, :]), writes=["convw"], dma=True)
                S.add("sp", lambda e: e.dma_start(out=rgvec[:], in_=rgvec_d[:, :]), writes=["rgvec"], dma=True)
                S.add("sp", lambda e: e.dma_start(out=pe2[:], in_=pe2_d[:, :]), writes=["pe2"], dma=True)
                S.add("sp", lambda e: e.dma_start(out=padmask[:], in_=padmask_d[:, :]), writes=["padmask"], dma=True)
                for L0 in range(2):
                    S.add("sp", lambda e, L0=L0: e.dma_start(out=xst[L0][:], in_=xs[L0 * 128:(L0 + 1) * 128, :]), writes=[f"xst{L0}"], dma=True)
                    S.add("dve", lambda e, L0=L0: e.tensor_copy(out=xb[L0][:], in_=xst[L0][:]), reads=[f"xst{L0}"], writes=[f"xb{L0}"])
                stB = xT[1][:].bitcast(F32)[:, 0:1280]
                for dc in range(8):
                    st = wst[0][:] if dc % 2 == 0 else stB
                    sk_ = "wst0" if dc % 2 == 0 else "xT1"
                    S.add("sp", lambda e, dc=dc, st=st: e.dma_start(out=st, in_=wA_d[dc * 128:(dc + 1) * 128, :]),
                          writes=[sk_], dma=True)
                    if dc % 2 == 0:
                        S.add("dve", lambda e, dc=dc, st=st: e.tensor_copy(out=wA[:, dc * 1280:(dc + 1) * 1280], in_=st),
                              reads=[sk_], writes=[f"wA{dc}"])
                    else:
                        S.add("act", lambda e, dc=dc, st=st: e.copy(out=wA[:, dc * 1280:(dc + 1) * 1280], in_=st),
                              reads=[sk_], writes=[f"wA{dc}"])
                wA_keys = [f"wA{dc}" for dc in range(8)]
                for nm, src, dst in (("BDa", BDa_d, BDa), ("BDx", BDx_d, BDx)):
                    S.add("sp", lambda e, src=src: e.dma_start(out=bdst[:], in_=src[:, :]), writes=["bdst"], dma=True)
                    S.add("dve", lambda e, dst=dst: e.tensor_copy(out=dst[:], in_=bdst[:]), reads=["bdst"], writes=[nm])
                def late_cw():
                    for nm, src, dst in (("BDWk", BDWk_d, BDWk), ("BDWv", BDWv_d, BDWv)):
                        for c in range(4):
                            st = wst[c % 2]
                            S.add("sp", lambda e, src=src, c=c, st=st: e.dma_start(out=st[:, 0:1024], in_=src[:, c * 1024:(c + 1) * 1024]),
                                  writes=["wst0"], dma=True)
                            S.add("dve", lambda e, dst=dst, c=c, st=st: e.tensor_copy(out=dst[:, c * 1024:(c + 1) * 1024], in_=st[:, 0:1024]),
                                  reads=["wst0"], writes=[nm])
                    S.add("dve", lambda e: e.tensor_copy(out=pe2b[:], in_=pe2[:]), reads=["pe2"], writes=["pe2b"])
                    S.add("sp", lambda e: e.dma_start(out=bdst[:], in_=ov_d[:, :]), writes=["bdst"], dma=True)
                    for ct in range(4):
                        for g in range(2):
                            o0 = (ct * 2 + g) * 193
                            S.add("dve", lambda e, ct=ct, o0=o0: e.tensor_copy(out=OVV[:, o0:o0 + 128], in_=bdst[:, ct * 128:(ct + 1) * 128]),
                                  reads=["bdst", "OVV"], writes=["OVV"])

                for kk in range(16):
                    S.add("dve", lambda e, kk=kk: e.tensor_scalar(out=dg[:, kk * 128:(kk + 1) * 128], in0=identf[:],
                                                                    scalar1=convw[:, kk:kk + 1], scalar2=None, op0=ALU.mult),
                          reads=["identf", "convw"], writes=["dg"])
                S.add("dve", lambda e: e.tensor_scalar(out=rgc[:, 0:8], in0=rgvec[:, 4:12], scalar1=0.5, scalar2=None, op0=ALU.mult),
                      reads=["rgvec"], writes=["rgc"])
                S.add("act", lambda e: e.activation(out=rgc[:, 16:20], in_=rgvec[:, 12:16], func=AF.Exp, scale=-1.0),
                      reads=["rgvec", "rgc"], writes=["rgc"])
                S.add("act", lambda e: e.activation(out=rgc[:, 16:20], in_=rgc[:, 16:20], func=AF.Ln, bias=1.0, scale=1.0),
                      reads=["rgc"], writes=["rgc"])
                S.add("dve", lambda e: e.tensor_scalar(out=rgc[:, 8:12], in0=rgc[:, 16:20], scalar1=-8.0, scalar2=None, op0=ALU.mult),
                      reads=["rgc"], writes=["rgc"])
                S.add("dve", lambda e: e.tensor_scalar(out=rgc[:, 12:16], in0=rgc[:, 16:20], scalar1=-4.0, scalar2=None, op0=ALU.mult),
                      reads=["rgc"], writes=["rgc"])
                def late_cb():
                    for w_, (nm, W) in enumerate((("BDWk", BDWk), ("BDWv", BDWv))):
                        for l in range(32):
                            S.add("pe", lambda e, W=W, l=l, w_=w_: e.matmul(ps[7][:, w_:w_ + 1], lhsT=W[:, l * 128:(l + 1) * 128],
                                                                             rhs=pe2b[:, w_ * 32 + l:w_ * 32 + l + 1],
                                                                             start=(l == 0), stop=(l == 31)),
                                  reads=[nm, "pe2b"], writes=["ps7"])
                    S.add("dve", lambda e: e.tensor_copy(out=cbk[:], in_=ps[7][:, 0:2]), reads=["ps7"], writes=["cbk"])


                def load_tile(L):
                    k = L % 2
                    S.add("sp", lambda e: e.dma_start(out=xst[k][:], in_=xs[L * 128:(L + 1) * 128, :]), writes=[f"xst{k}"], dma=True)
                    S.add("dve", lambda e: e.tensor_copy(out=xb[k][:], in_=xst[k][:]), reads=[f"xst{k}"], writes=[f"xb{k}"])

                def transpose_tile(L):
                    k = L % 2
                    G, t = divmod(L, 4)
                    pb = psb[k]
                    for dc in range(8):
                        S.add("pe", lambda e, dc=dc: e.transpose(out=pb[:, dc * 128:(dc + 1) * 128], in_=xb[k][:, dc * 128:(dc + 1) * 128], identity=ident[:]),
                              reads=[f"xb{k}", "ident"], writes=[f"ps{k}"])
                    dst = xT[G % 2][:].rearrange("p (c n) -> p c n", c=8)[:, :, t * 128:(t + 1) * 128]
                    src = pb[:].rearrange("p (c n) -> p c n", c=8)
                    if L % 2 == 0:
                        S.add("dve", lambda e: e.tensor_copy(out=dst, in_=src), reads=[f"ps{k}"], writes=[f"xT{G % 2}"])
                    else:
                        S.add("act", lambda e: e.copy(out=dst, in_=src), reads=[f"ps{k}"], writes=[f"xT{G % 2}"])

                def vproj_tile(L):
                    G, t = divmod(L, 4)
                    X = xT[G % 2]
                    for dc in range(8):
                        S.add("pe", lambda e, dc=dc: e.matmul(ps[2][:, 0:256], lhsT=X[:, dc * 512 + t * 128: dc * 512 + (t + 1) * 128],
                                                               rhs=wA[:, dc * 1280 + 1024: dc * 1280 + 1280], start=(dc == 0), stop=(dc == 7)),
                              reads=[f"xT{G % 2}", f"wA{dc}"], writes=["ps2"])
                    for vi, (nm, V) in enumerate((("Vs", Vs), ("Vw", Vw))):
                        dst = V[:, L * 130:(L + 1) * 130].rearrange("p (g c) -> p g c", g=2)[:, :, 0:64]
                        src = ps[2][:, vi * 128:(vi + 1) * 128].rearrange("p (g c) -> p g c", g=2)
                        S.add("dve", lambda e, dst=dst, src=src: e.tensor_copy(out=dst, in_=src), reads=["ps2"], writes=[nm])

                def fproj(G, m):
                    X = xT[G % 2]
                    bank = 3 + (m % 2)
                    for dc in range(8):
                        S.add("pe", lambda e, dc=dc: e.matmul(ps[bank][:, :], lhsT=wA[:, dc * 1280 + m * 128: dc * 1280 + (m + 1) * 128],
                                                               rhs=X[:, dc * 512:(dc + 1) * 512], start=(dc == 0), stop=(dc == 7)),
                              reads=[f"xT{G % 2}", f"wA{dc}"], writes=[f"ps{bank}"])
                    if m == 0:
                        o = 16 + (G % 4) * 512
                        S.add("act", lambda e: e.copy(out=kraw[:, o:o + 512], in_=ps[bank][:, :]), reads=[f"ps{bank}"], writes=["kraw"])
                    elif m == 1:
                        o = 16 + (G % 4) * 512
                        S.add("act", lambda e: e.copy(out=vraw[:, o:o + 512], in_=ps[bank][:, :]), reads=[f"ps{bank}"], writes=["vraw"])
                    elif m == 2:
                        S.add("dve", lambda e: e.tensor_copy(out=KsT[:, G * 512:(G + 1) * 512], in_=ps[bank][:, :]), reads=[f"ps{bank}"], writes=["KsT"])
                    elif m == 3:
                        S.add("dve", lambda e: e.tensor_copy(out=KwT[:, G * 512:(G + 1) * 512], in_=ps[bank][:, :]), reads=[f"ps{bank}"], writes=["KwT"])
                    else:
                        ct = m - 4
                        p = G % 2
                        if G > 0:
                            S.add("pool", lambda e: e.tensor_copy(out=rxb[p][ct][:, 0:3], in_=rxb[1 - p][ct][:, 512:515]),
                                  reads=[f"rxb{1 - p}_{ct}"], writes=[f"rxb{p}_{ct}"])
                        S.add("act", lambda e: e.copy(out=rxb[p][ct][:, 3:515], in_=ps[bank][:, :]), reads=[f"ps{bank}"], writes=[f"rxb{p}_{ct}"])

                def rnn_conv(G, ct):
                    p = G % 2
                    for k in range(4):
                        S.add("pe", lambda e, k=k: e.matmul(ps[5][:, :], lhsT=dg[:, (k * 4 + ct) * 128:(k * 4 + ct + 1) * 128],
                                                             rhs=rxb[p][ct][:, k:k + 512], start=(k == 0), stop=(k == 3)),
                              reads=["dg", f"rxb{p}_{ct}"], writes=["ps5"])
                    r = ct % NR
                    S.add("act", lambda e: e.activation(out=xcb[r][:], in_=ps[5][:, :], func=AF.Identity, bias=rgvec[:, ct:ct + 1], scale=1.0),
                          reads=["ps5", "rgvec"], writes=[f"xcb{r}"])
                    S.add("act", lambda e: e.activation(out=xcf[r][:], in_=ps[5][:, :], func=AF.Identity, bias=rgvec[:, ct:ct + 1], scale=1.0),
                          reads=["ps5", "rgvec"], writes=[f"xcf{r}"])

                def rnn_gates_front(G, cts):
                    for ct in cts:
                        r = ct % NR
                        S.add("pe", lambda e, ct=ct, r=r: e.matmul(ps[6][:, :], lhsT=BDa[:, ct * 128:(ct + 1) * 128], rhs=xcb[r][:], start=True, stop=True),
                              reads=["BDa", f"xcb{r}"], writes=["ps6"])
                        S.add("pe", lambda e, ct=ct, r=r: e.matmul(ps[7][:, :], lhsT=BDx[:, ct * 128:(ct + 1) * 128], rhs=xcb[r][:], start=True, stop=True),
                              reads=["BDx", f"xcb{r}"], writes=["ps7"])
                        S.add("act", lambda e, ct=ct, r=r: e.activation(out=thr_[r][:], in_=ps[6][:, :], func=AF.Tanh, bias=rgc[:, ct:ct + 1], scale=0.5),
                              reads=["ps6", "rgc"], writes=[f"thr{r}"])
                        S.add("act", lambda e, ct=ct, r=r: e.activation(out=thx[r][:], in_=ps[7][:, :], func=AF.Tanh, bias=rgc[:, 4 + ct:5 + ct], scale=0.5),
                              reads=["ps7", "rgc"], writes=[f"thx{r}"])

                def rnn_gates(G, cts):
                    for ct in cts:
                        r = ct % NR
                        S.add("act", lambda e, ct=ct, r=r: e.activation(out=av[r][:], in_=thr_[r][:], func=AF.Exp, bias=rgc[:, 12 + ct:13 + ct], scale=rgc[:, 12 + ct:13 + ct]),
                              reads=[f"thr{r}", "rgc"], writes=[f"av{r}"])
                        S.add("act", lambda e, ct=ct, r=r: e.activation(out=thr_[r][:], in_=thr_[r][:], func=AF.Exp, bias=rgc[:, 8 + ct:9 + ct], scale=rgc[:, 8 + ct:9 + ct]),
                              reads=[f"thr{r}", "rgc"], writes=[f"thr{r}"])
                    for ct in cts:
                        r = ct % NR
                        S.add("act", lambda e, r=r: e.activation(out=thr_[r][:], in_=thr_[r][:], func=AF.Sqrt, bias=1.0, scale=-1.0),
                              reads=[f"thr{r}"], writes=[f"thr{r}"])
                    for ct in cts:
                        r = ct % NR
                        S.add("dve", lambda e, r=r: e.scalar_tensor_tensor(out=thx[r][:], in0=thx[r][:], scalar=1.0, in1=xcf[r][:], op0=ALU.add, op1=ALU.mult),
                              reads=[f"thx{r}", f"xcf{r}"], writes=[f"thx{r}"])

                def rnn_scan(G, cts):
                    for ct in cts:
                        r = ct % NR
                        S.add("dve", lambda e, r=r: e.scalar_tensor_tensor(out=thx[r][:], in0=thr_[r][:], scalar=0.5, in1=thx[r][:], op0=ALU.mult, op1=ALU.mult),
                              reads=[f"thx{r}", f"thr{r}"], writes=[f"thx{r}"])
                        if G == 0:
                            S.add("dve", lambda e, r=r: e.tensor_tensor(out=thx[r][:], in0=thx[r][:], in1=padmask[:], op=ALU.mult),
                                  reads=[f"thx{r}", "padmask"], writes=[f"thx{r}"])
                        S.add("dve", lambda e, ct=ct, r=r: e.tensor_tensor_scan(out=hb[r][:], data0=av[r][:], data1=thx[r][:], initial=carry[:, ct:ct + 1],
                                                                            op0=ALU.mult, op1=ALU.add),
                              reads=[f"av{r}", f"thx{r}", "carry"], writes=[f"hb{r}"])
                        S.add("dve", lambda e, ct=ct, r=r: e.tensor_copy(out=carry[:, ct:ct + 1], in_=hb[r][:, 511:512]), reads=[f"hb{r}", "carry"], writes=["carry"])
                        S.add("pool", lambda e, ct=ct, r=r: e.tensor_copy(out=hown[:, ct * 2048 + G * 128: ct * 2048 + (G + 1) * 128], in_=hb[r][:, 384:512]),
                              reads=[f"hb{r}"], writes=["hown"])

                def compress(SG):
                    for w_, (nm, W, raw, rawk) in enumerate((("BDWk", BDWk, kraw, "kraw"), ("BDWv", BDWv, vraw, "vraw"))):
                        bank = 3 + w_
                        for l in range(32):
                            S.add("pe", lambda e, W=W, raw=raw, l=l, bank=bank: e.matmul(ps[bank][:, 0:128], lhsT=W[:, l * 128:(l + 1) * 128],
                                                                                       rhs=raw[:, l:l + 2048:16], start=(l == 0), stop=(l == 31)),
                                  reads=[nm, rawk], writes=[f"ps{bank}"])
                        if w_ == 0:
                            S.add("act", lambda e: e.activation(out=KcT[:, SG * 128:(SG + 1) * 128], in_=ps[3][:, 0:128], func=AF.Identity,
                                                                bias=cbk[:, 0:1], scale=1.0),
                                  reads=["ps3", "cbk"], writes=["KcT"])
                        else:
                            S.add("act", lambda e: e.activation(out=vcs[:], in_=ps[4][:, 0:128], func=AF.Identity, bias=cbk[:, 1:2], scale=1.0),
                                  reads=["ps4", "cbk"], writes=["vcs"])
                            S.add("pe", lambda e: e.transpose(out=psb[4][:, 512:640], in_=vcs[:], identity=ident[:]),
                                  reads=["vcs", "ident", "ps4"], writes=["ps4"])
                            dst = OVV[:, SG * 386:(SG + 1) * 386].rearrange("p (g c) -> p g c", g=2)[:, :, 128:192]
                            src = psb[4][:, 512:640].rearrange("p (g c) -> p g c", g=2)
                            S.add("dve", lambda e, dst=dst, src=src: e.tensor_copy(out=dst, in_=src), reads=["ps4", "OVV"], writes=["OVV"])
                        S.add("pool", lambda e, raw=raw: e.tensor_copy(out=raw[:, 0:16], in_=raw[:, 2048:2064]), reads=[rawk], writes=[rawk])

                for t in range(4):
                    transpose_tile(t)
                    load_tile(t + 2)
                late_cw()
                for t in range(4):
                    vproj_tile(t)
                for G in range(18):
                    rn = [None] * 8
                    if 1 <= G <= 16:
                        rn = [("c", 0), ("c", 1), ("f", (0,)), ("fg", ((1,), (0, 1))), ("c", 2), ("cs", (3, (0, 1))), ("f", (2,)), ("fg", ((3,), (2, 3)))]
                    for m in range(8):
                        if G < 16:
                            fproj(G, m)
                        if m == 1 and G >= 2:
                            rnn_scan(G - 2, (2, 3))
                        if rn[m] is not None:
                            kind, ct = rn[m]
                            if kind == "c":
                                rnn_conv(G - 1, ct)
                            elif kind == "cs":
                                rnn_conv(G - 1, ct[0])
                                rnn_scan(G - 1, ct[1])
                            elif kind == "f":
                                rnn_gates_front(G - 1, ct)
                            else:
                                rnn_gates_front(G - 1, ct[0])
                                rnn_gates(G - 1, ct[1])
                        if G + 1 < 16:
                            L = 4 * (G + 1) + m // 2
                            if m % 2 == 0:
                                transpose_tile(L)
                                if L + 2 < NT:
                                    load_tile(L + 2)
                            else:
                                vproj_tile(L)
                    if G == 0:
                        late_cb()
                    if G < 16 and G % 4 == 3:
                        compress(G // 4)
                if "p1" in dbg:
                    es.dump_tmp = wst[0]
                    dump(S, es, "KsT", KsT, BF16)
                    dump(S, es, "KwT", KwT, BF16)
                    dump(S, es, "Vs", Vs, BF16)
                    dump(S, es, "KcT", KcT, BF16)
                    dump(S, es, "OVV", OVV, BF16)
                    dump(S, es, "hown", hown, BF16)
                S.drain()
                with nc.Block() as block:
                    base = S.emit(block, sems)
            if "stop1" in dbg:
                h_scope.close()
                return nc

            r_scope = ExitStack()

            def rsb(name, shape, dt):
                return r_scope.enter_context(nc.sbuf_tensor("sb_" + name, list(shape), dt, side="right"))
            rnnT = rsb("rnnT", [128, 4 * 2048], BF16)
            gates = rsb("gates", [128, NOWN * 24], F32)
            attnT = rsb("attnT", [128, 4 * 2048], BF16)
            q_scope = ExitStack()
            QT = [q_scope.enter_context(nc.sbuf_tensor(f"sb_QT{g}", [128, NOWN * 512], BF16, side="right")) for g in range(2)]

            with ExitStack() as es:
                def sb(name, shape, dt):
                    return es.enter_context(nc.sbuf_tensor("p2a_" + name, list(shape), dt))
                S = Sched(base)
                wC = sb("wC", [128, 8 * 1048], BF16)
                wst = sb("wst2", [128, 1048], F32)
                wstb = sb("wst2b", [128, 1048], F32)
                xst = [sb(f"xst{k}", [128, 1024], F32) for k in range(2)]
                xb = [sb(f"xb{k}", [128, 1024], BF16) for k in range(2)]
                xTo = [sb("xTo0", [128, 8 * 512], BF16)] * 2
                gel = [sb(f"gel{k}", [128, 512], F32) for k in range(2)]
                gf = sb("gf", [128, 4 * 512], F32)
                onesf = sb("onesf", [128, 1], F32)
                gtmp = sb("gtmp", [128, 24], F32)
                S.add("pool", lambda e: e.memset(onesf[:], 1.0), writes=["onesf"])
                for g in range(2):
                    S.add("pool", lambda e, g=g: e.memset(QT[g][:], 0.0), writes=["QT"])
                for dc in range(8):
                    wb = wst if dc % 2 == 0 else wstb
                    S.add("sp", lambda e, dc=dc, wb=wb: e.dma_start(out=wb[:], in_=wC_d[dc * 128:(dc + 1) * 128, :]), writes=[f"wst{dc % 2}"], dma=True)
                    S.add("dve" if dc % 2 == 0 else "pool", lambda e, dc=dc, wb=wb: e.tensor_copy(out=wC[:, dc * 1048:(dc + 1) * 1048], in_=wb[:]), reads=[f"wst{dc % 2}"], writes=[f"wC{dc}"])
                wC_keys = [f"wC{dc}" for dc in range(8)]

                def load_own(i):
                    k = i % 2
                    L = 4 * i + 3
                    S.add("sp", lambda e: e.dma_start(out=xst[k][:], in_=xs[L * 128:(L + 1) * 128, :]), writes=[f"xst{k}"], dma=True)
                    S.add("dve", lambda e: e.tensor_copy(out=xb[k][:], in_=xst[k][:]), reads=[f"xst{k}"], writes=[f"xb{k}"])

                load_own(0)
                load_own(1)
                def do_I(I):
                    X = xTo[I % 2]
                    for t in range(4):
                        i = 4 * I + t
                        k = i % 2
                        pb = psb[k]
                        for dc in range(8):
                            S.add("pe", lambda e, dc=dc, k=k, pb=pb: e.transpose(out=pb[:, dc * 128:(dc + 1) * 128], in_=xb[k][:, dc * 128:(dc + 1) * 128], identity=ident[:]),
                                  reads=[f"xb{k}", "ident"], writes=[f"ps{k}"])
                        dst = X[:].rearrange("p (c n) -> p c n", c=8)[:, :, t * 128:(t + 1) * 128]
                        src = pb[:].rearrange("p (c n) -> p c n", c=8)
                        S.add("act", lambda e, dst=dst, src=src: e.copy(out=dst, in_=src), reads=[f"ps{k}"], writes=["xTo0"])
                        if i + 2 < NOWN:
                            load_own(i + 2)
                    for t in range(4):
                        i = 4 * I + t
                        for dc in range(8):
                            S.add("pe", lambda e, dc=dc, t=t: e.matmul(ps[2][:, 0:24], lhsT=X[:, dc * 512 + t * 128: dc * 512 + (t + 1) * 128],
                                                                     rhs=wC[:, dc * 1048 + 1024: dc * 1048 + 1048], start=(dc == 0), stop=(dc == 7)),
                                  reads=["xTo0", f"wC{dc}"], writes=["ps2"])
                        S.add("act", lambda e: e.activation(out=gtmp[:], in_=ps[2][:, 0:24], func=AF.Tanh, scale=0.5), reads=["ps2"], writes=["gtmp"])
                        S.add("dve", lambda e, i=i: e.tensor_scalar(out=gates[:, i * 24:(i + 1) * 24], in0=gtmp[:], scalar1=0.5, scalar2=0.5, op0=ALU.mult, op1=ALU.add),
                              reads=["gtmp"], writes=["gates"])
                    for m in range(8):
                        bank = 3 + (m % 2)
                        for dc in range(8):
                            S.add("pe", lambda e, dc=dc, m=m, bank=bank: e.matmul(ps[bank][:, :], lhsT=wC[:, dc * 1048 + m * 128: dc * 1048 + (m + 1) * 128],
                                                                               rhs=X[:, dc * 512:(dc + 1) * 512], start=(dc == 0), stop=(dc == 7)),
                                  reads=["xTo0", f"wC{dc}"], writes=[f"ps{bank}"])
                        if m < 4:
                            for g in range(2):
                                gpp = slice(64 * g, 64 * g + 64)
                                dst = QT[g][gpp, 4 * I * 512:(4 * I + 4) * 512].rearrange("p (t r q) -> p t r q", t=4, r=4)[:, :, m, :]
                                src = ps[bank][gpp, :].rearrange("p (t q) -> p t q", t=4)
                                S.add("act", lambda e, dst=dst, src=src: e.activation(out=dst, in_=src, func=AF.Copy, scale=0.125), reads=[f"ps{bank}"], writes=["QT"])
                        else:
                            ct = m - 4
                            gb = gel[ct % 2]
                            S.add("act", lambda e, gb=gb, bank=bank: e.activation(out=gb[:], in_=ps[bank][:, :], func=AF.Gelu_apprx_tanh),
                                  reads=[f"ps{bank}"], writes=[f"gel{ct % 2}"])
                            S.add("dve", lambda e, gb=gb, ct=ct: e.tensor_tensor(out=gf[:, ct * 512:(ct + 1) * 512], in0=gb[:],
                                                                               in1=hown[:, ct * 2048 + I * 512: ct * 2048 + (I + 1) * 512], op=ALU.mult),
                                  reads=[f"gel{ct % 2}", "hown"], writes=[f"gf{ct}"])
                            S.add("act", lambda e, ct=ct: e.copy(out=rnnT[:, ct * 2048 + I * 512: ct * 2048 + (I + 1) * 512], in_=gf[:, ct * 512:(ct + 1) * 512]),
                                  reads=[f"gf{ct}"], writes=["rnnT"])
                            S.add("pool", lambda e, ct=ct: e.tensor_tensor(out=gf[:, ct * 512:(ct + 1) * 512], in0=gf[:, ct * 512:(ct + 1) * 512],
                                                                          in1=gf[:, ct * 512:(ct + 1) * 512], op=ALU.mult),
                                  reads=[f"gf{ct}"], writes=[f"gf{ct}"])
                    for t in range(4):
                        i = 4 * I + t
                        for ct in range(4):
                            S.add("pe", lambda e, i=i, t=t, ct=ct: e.matmul(ps[5][:, i:i + 1], lhsT=gf[:, ct * 512 + t * 128: ct * 512 + (t + 1) * 128],
                                                                         rhs=onesf[:, 0:1], start=(ct == 0), stop=(ct == 3)),
                                  reads=[f"gf{ct}", "onesf"], writes=["ps5"])
                for I in range(4):
                    do_I(I)
                S.add("dve", lambda e: e.tensor_copy(out=ssr[:], in_=ps[5][:, 0:NOWN]), reads=["ps5"], writes=["ssr"])
                if "p2a" in dbg:
                    es.dump_tmp = wst
                    dump(S, es, "QT", QT[0], BF16)
                    dump(S, es, "rnnT", rnnT, BF16)
                    dump(S, es, "gates", gates, F32)
                    dump(S, es, "ssr", ssr, F32)
                S.drain()
                with nc.Block() as block:
                    base = S.emit(block, sems)
            h_scope.close()
            if "stop2a" in dbg:
                q_scope.close()
                r_scope.close()
                return nc

            with ExitStack() as es:
                def sb(name, shape, dt):
                    return es.enter_context(nc.sbuf_tensor("p2b_" + name, list(shape), dt))
                S = Sched(base)
                tabs = sb("tabs", [128, NTAB * 1024], BF16)
                Eb = sb("Eb", [128, SEQ], BF16)
                stg = sb("stg", [128, 2048], F32)
                keybias = sb("keybias", [128, NT], F32)
                cbias = sb("cbias", [128, 4], F32)
                cb = sb("cb", [128, 8], F32)
                cbm = sb("cbm", [128, 8], F32)
                selb = [sb(f"selb{k}", [128, 128], F32) for k in range(2)]
                Pc = [sb(f"Pc{k}", [128, 512], BF16) for k in range(4)]
                NP = 4
                Pp = [sb(f"Pp{k}", [128, 512], BF16) for k in range(NP)]
                R0 = [sb(f"R0_{g}", [128, 512], BF16) for g in range(2)]
                Rc = [sb(f"Rc_{g}", [128, 512], BF16) for g in range(2)]
                score = sb("score", [128, 128], F32)
                work = sb("work", [128, 128], F32)
                m16 = sb("m16", [128, 16], F32)
                selm = sb("selm", [128, 128], BF16)
                zr = sb("zr", [128, 4], F32)
                coef = sb("coef", [128, 4], F32)
                OTs = [sb(f"OTs{k}", [128, 512], F32) for k in range(2)]
                attn = [sb(f"attn{k}", [128, 512], F32) for k in range(2)]
                attnb = sb("attnb", [128, 512], BF16)
                junk = sb("junk", [128, 512], BF16)
                ssa = sb("ssa", [128, 2], F32)
                nhalf = sb("nhalf", [128, 1], F32)
                seldbg = sb("seldbg", [128, NOWN * 2 * 128], BF16) if "p2b" in dbg else None

                S.add("pool", lambda e: e.memset(nhalf[:], -0.5), writes=["nhalf"])
                S.add("sp", lambda e: e.dma_start(out=keybias[:], in_=keybias_d[:, :]), writes=["keybias"], dma=True)
                S.add("sp", lambda e: e.dma_start(out=cbias[:], in_=cbias_d[:, :]), writes=["cbias"], dma=True)
                S.add("sp", lambda e: e.dma_start(out=cb[:], in_=cb_d[:, :]), writes=["cb"], dma=True)
                S.add("dve", lambda e: e.tensor_scalar(out=cbm[:], in0=cb[:], scalar1=MASKV, scalar2=None, op0=ALU.add), reads=["cb"], writes=["cbm"])
                for tb in range(NTAB):
                    hh = tb % 2
                    S.add("sp", lambda e, tb=tb, hh=hh: e.dma_start(out=stg[:, hh * 1024:(hh + 1) * 1024], in_=tabs_d[tb, :, :]), writes=[f"stg{hh}"], dma=True)
                    S.add("pool" if hh == 0 else "dve", lambda e, tb=tb, hh=hh: e.tensor_copy(out=tabs[:, tb * 1024:(tb + 1) * 1024], in_=stg[:, hh * 1024:(hh + 1) * 1024]),
                          reads=[f"stg{hh}"], writes=["tabs"])
                for c in range(8):
                    hh = c % 2
                    S.add("sp", lambda e, c=c, hh=hh: e.dma_start(out=stg[:, hh * 1024:(hh + 1) * 1024], in_=E_d[:, c * 1024:(c + 1) * 1024]), writes=[f"stg{hh}"], dma=True)
                    S.add("pool" if hh == 0 else "dve", lambda e, c=c, hh=hh: e.tensor_copy(out=Eb[:, c * 1024:(c + 1) * 1024], in_=stg[:, hh * 1024:(hh + 1) * 1024]),
                          reads=[f"stg{hh}"], writes=["Eb"])

                sctr = [0]
                pctr = [0]

                SB = (0, 1, 7)

                def run_jobs(jobs):
                    LAG = 2
                    q = []
                    for job in jobs + [None] * LAG:
                        if job is not None:
                            sb_ = SB[sctr[0] % 3]
                            sctr[0] += 1
                            pp = pctr[0] % NP
                            pctr[0] += 1
                            n = len(job["mms"])
                            for q_, (lh, rh, rds) in enumerate(job["mms"]):
                                S.add("pe", lambda e, lh=lh, rh=rh, q_=q_, n=n, sb_=sb_: e.matmul(ps[sb_][:, :], lhsT=lh, rhs=rh, start=(q_ == 0), stop=(q_ == n - 1)),
                                      reads=rds, writes=[f"ps{sb_}"])
                            S.add("act", lambda e, sb_=sb_, pp=pp, bias=job["bias"]: e.activation(out=Pp[pp][:], in_=ps[sb_][:, :], func=AF.Exp, bias=bias, scale=1.0),
                                  reads=[f"ps{sb_}", "keybias"], writes=[f"Pp{pp}"])
                            job["pp"] = pp
                        q.append(job)
                        if len(q) > LAG:
                            pend = q.pop(0)
                            if pend is not None:
                                ob = pend["obank"]
                                S.add("pe", lambda e, pend=pend, ob=ob: e.matmul(ps[ob][:, :], lhsT=pend["v"], rhs=Pp[pend["pp"]][:], start=pend["first"], stop=pend["last"]),
                                      reads=[f"Pp{pend['pp']}", pend["vkey"]], writes=[f"ps{ob}"])

                def fin_copy(obank, bri):
                    o = OTs[bri % 2]
                    S.add("dve", lambda e: e.tensor_copy(out=o[0:65, :], in_=ps[obank][0:65, :]), reads=[f"ps{obank}"], writes=[f"OTs{bri % 2}"])

                def fin_late(i, g, bri, A):
                    o = OTs[bri % 2]
                    for r in range(4):
                        S.add("pe", lambda e, r=r: e.transpose(out=ps[6][:, r * 65:(r + 1) * 65], in_=o[0:65, r * 128:(r + 1) * 128], identity=identf[0:65, 0:65]),
                              reads=[f"OTs{bri % 2}", "identf"], writes=["ps6"])
                    S.add("dve", lambda e: e.tensor_copy(out=zr[:], in_=ps[6][:, 64:260:65]), reads=["ps6"], writes=["zr"])
                    S.add("dve", lambda e: e.reciprocal(out=zr[:], in_=zr[:]), reads=["zr"], writes=["zr"])
                    g0 = i * 24 + g * 12 + bri
                    S.add("dve", lambda e: e.tensor_tensor(out=coef[:], in0=zr[:], in1=gates[:, g0:g0 + 10:3], op=ALU.mult), reads=["zr", "gates"], writes=["coef"])
                    for r in range(4):
                        h = 4 * g + r
                        S.add("dve", lambda e, r=r, h=h: e.scalar_tensor_tensor(out=A[:, h * 64:(h + 1) * 64], in0=ps[6][:, r * 65:r * 65 + 64], scalar=coef[:, r:r + 1],
                                                                               in1=A[:, h * 64:(h + 1) * 64], op0=ALU.mult, op1=ALU.add),
                              reads=["ps6", "coef", f"attn{i % 2}"], writes=[f"attn{i % 2}"])

                def stage_C1(i, g, A, sbk):
                    if True:
                        Qi = QT[g][:, i * 512:(i + 1) * 512]
                        nct = i // 4 + 1
                        for ct in range(nct):
                            sb_ = SB[sctr[0] % 3]
                            sctr[0] += 1
                            tid = (T_N0 + i % 4) if ct == nct - 1 else T_C
                            S.add("pe", lambda e, ct=ct, sb_=sb_: e.matmul(ps[sb_][:, :], lhsT=KcT[:, ct * 128:(ct + 1) * 128], rhs=Qi, start=True, stop=False),
                                  reads=["KcT", "QT"], writes=[f"ps{sb_}"])
                            S.add("pe", lambda e, tid=tid, sb_=sb_: e.matmul(ps[sb_][:, :], lhsT=ident[:, :], rhs=tabs[:, tid * 1024 + g * 512: tid * 1024 + (g + 1) * 512],
                                                                          start=False, stop=True),
                                  reads=["ident", "tabs"], writes=[f"ps{sb_}"])
                            S.add("act", lambda e, ct=ct, sb_=sb_: e.activation(out=Pc[ct][:], in_=ps[sb_][:, :], func=AF.Exp, bias=cbias[:, ct:ct + 1], scale=1.0),
                                  reads=[f"ps{sb_}", "cbias"], writes=[f"Pc{ct}"])
                        for r in range(4):
                            ub = 2 + r // 2
                            c0 = (r % 2) * 193
                            for ct in range(nct):
                                S.add("pe", lambda e, r=r, ct=ct, ub=ub, c0=c0: e.matmul(ps[ub][:, c0:c0 + 193], lhsT=Pc[ct][:, r * 128:(r + 1) * 128],
                                                                                     rhs=OVV[:, (ct * 2 + g) * 193:(ct * 2 + g + 1) * 193], start=(ct == 0), stop=(ct == nct - 1)),
                                      reads=[f"Pc{ct}", "OVV"], writes=[f"ps{ub}"])
                        for hb_ in range(2):
                            S.add("dve", lambda e, hb_=hb_: e.tensor_scalar(out=zr[:, 2 * hb_:2 * hb_ + 2], in0=ps[2 + hb_][:, 192:386:193], scalar1=1e-30, scalar2=None, op0=ALU.max),
                                  reads=[f"ps{2 + hb_}", "zr"], writes=["zr"])
                        S.add("dve", lambda e: e.reciprocal(out=zr[:], in_=zr[:]), reads=["zr"], writes=["zr"])
                        for r in range(4):
                            ub = 2 + r // 2
                            c0 = (r % 2) * 193
                            if r == 0:
                                S.add("dve", lambda e, ub=ub, c0=c0: e.tensor_scalar(out=score[:], in0=ps[ub][:, c0:c0 + 128], scalar1=zr[:, 0:1], scalar2=None, op0=ALU.mult),
                                      reads=[f"ps{ub}", "zr"], writes=["score"])
                            else:
                                S.add("dve", lambda e, ub=ub, c0=c0, r=r: e.scalar_tensor_tensor(out=score[:], in0=ps[ub][:, c0:c0 + 128], scalar=zr[:, r:r + 1], in1=score[:],
                                                                                             op0=ALU.mult, op1=ALU.add),
                                      reads=[f"ps{ub}", "zr", "score"], writes=["score"])
                        S.add("dve", lambda e, sbk=sbk: e.tensor_tensor(out=score[:], in0=score[:], in1=sbk[:], op=ALU.add), reads=["score", f"selb{i % 2}"], writes=["score"])
                        g0 = i * 24 + g * 12
                        S.add("dve", lambda e, g0=g0: e.tensor_tensor(out=coef[:], in0=zr[:], in1=gates[:, g0:g0 + 10:3], op=ALU.mult), reads=["zr", "gates"], writes=["coef"])
                        for r in range(4):
                            ub = 2 + r // 2
                            c0 = (r % 2) * 193
                            h = 4 * g + r
                            S.add("dve", lambda e, ub=ub, c0=c0, r=r, h=h: e.tensor_scalar(out=A[:, h * 64:(h + 1) * 64], in0=ps[ub][:, c0 + 128:c0 + 192], scalar1=coef[:, r:r + 1],
                                                                                       scalar2=None, op0=ALU.mult),
                                  reads=[f"ps{ub}", "coef"], writes=[f"attn{i % 2}"])
                        S.add("dve", lambda e: e.max(out=m16[:, 0:8], in_=score[:]), reads=["score"], writes=["m16"])
                        S.add("dve", lambda e: e.match_replace(out=work[:], in_to_replace=m16[:, 0:8], in_values=score[:], imm_value=-3.0e38), reads=["score", "m16"], writes=["work"])
                        S.add("dve", lambda e: e.max(out=m16[:, 8:16], in_=work[:]), reads=["work", "m16"], writes=["m16"])
                        S.add("dve", lambda e: e.tensor_scalar(out=selm[:], in0=score[:], scalar1=m16[:, 15:16], scalar2=None, op0=ALU.is_ge), reads=["score", "m16"], writes=["selm"])
                        if seldbg is not None:
                            S.add("pool", lambda e, i=i, g=g: e.tensor_copy(out=seldbg[:, (i * 2 + g) * 128:(i * 2 + g + 1) * 128], in_=selm[:]), reads=["selm"], writes=["seldbg"])

                def stage_C2(i, g):
                    if True:
                        S.add("pe", lambda e: e.transpose(out=psb[6][:, 0:128], in_=selm[:], identity=ident[:]), reads=["selm", "ident"], writes=["ps6"])
                        for r in range(4):
                            h = 4 * g + r
                            S.add("dve", lambda e, r=r, h=h: e.tensor_scalar(out=Rc[g][:, r * 128:(r + 1) * 128], in0=psb[6][:, 0:128], scalar1=-MASKV, scalar2=cbm[:, h:h + 1],
                                                                           op0=ALU.mult, op1=ALU.add),
                                  reads=["ps6", "cbm"], writes=[f"Rc{g}"])
                            S.add("dve", lambda e, r=r: e.tensor_scalar(out=R0[g][:, r * 128:(r + 1) * 128], in0=psb[6][:, 0:128], scalar1=-MASKV, scalar2=MASKV,
                                                                      op0=ALU.mult, op1=ALU.add),
                                  reads=["ps6"], writes=[f"R0{g}"])

                def stage_S(i, g):
                    if True:
                        Qi = QT[g][:, i * 512:(i + 1) * 512]
                        jobs = []
                        Lmax = 4 * i + 3
                        for L in range(Lmax + 1):
                            near = L >= Lmax - 1
                            mms = [(KsT[:, L * 128:(L + 1) * 128], Qi, ["KsT", "QT"]),
                                   (Eb[:, L * 128:(L + 1) * 128], (R0 if near else Rc)[g][:], ["Eb", f"R0{g}" if near else f"Rc{g}"])]
                            if near:
                                tid = T_D if L == Lmax else T_O1
                                mms.append((ident[:, :], tabs[:, tid * 1024 + g * 512: tid * 1024 + (g + 1) * 512], ["ident", "tabs"]))
                            jobs.append(dict(mms=mms, bias=keybias[:, L:L + 1], v=Vs[:, L * 130 + g * 65: L * 130 + g * 65 + 128], vkey="Vs", obank=4,
                                             first=(L == 0), last=(L == Lmax)))
                        run_jobs(jobs)

                def stage_W(i, g):
                    if True:
                        Qi = QT[g][:, i * 512:(i + 1) * 512]
                        Lmax = 4 * i + 3
                        jobs = []
                        Ls = [L for L in range(Lmax - 4, Lmax + 1) if L >= 0]
                        for L in Ls:
                            rel = L - Lmax
                            tid = {0: T_D, -1: T_O1, -2: T_C, -3: T_C, -4: T_W4}[rel]
                            mms = [(KwT[:, L * 128:(L + 1) * 128], Qi, ["KwT", "QT"]),
                                   (ident[:, :], tabs[:, tid * 1024 + g * 512: tid * 1024 + (g + 1) * 512], ["ident", "tabs"])]
                            jobs.append(dict(mms=mms, bias=keybias[:, L:L + 1], v=Vw[:, L * 130 + g * 65: L * 130 + g * 65 + 128], vkey="Vw", obank=5,
                                             first=(L == Ls[0]), last=(L == Lmax)))
                        run_jobs(jobs)

                def block_done(i):
                    A = attn[i % 2]
                    S.add("act", lambda e, A=A: e.activation(out=junk[:], in_=A[:], func=AF.Square, accum_out=ssa[:, 0:1]), reads=[f"attn{i % 2}"], writes=["junk", "ssa"])
                    S.add("dve", lambda e: e.tensor_scalar(out=ssa[:, 1:2], in0=ssa[:, 0:1], scalar1=1.0 / 512.0, scalar2=RMS_EPS, op0=ALU.mult, op1=ALU.add), reads=["ssa"], writes=["ssa2"])
                    S.add("act", lambda e: e.activation(out=ssa[:, 1:2], in_=ssa[:, 1:2], func=AF.Ln), reads=["ssa2"], writes=["ssa2"])
                    S.add("act", lambda e, i=i: e.activation(out=rstd_a[:, i:i + 1], in_=ssa[:, 1:2], func=AF.Exp, scale=-0.5), reads=["ssa2"], writes=["rstd_a"])
                    S.add("pool", lambda e, A=A: e.tensor_copy(out=attnb[:], in_=A[:]), reads=[f"attn{i % 2}"], writes=["attnb"])
                    for fc in range(4):
                        S.add("pe", lambda e, fc=fc: e.transpose(out=psb[6][:, 512 + fc * 128:512 + (fc + 1) * 128], in_=attnb[:, fc * 128:(fc + 1) * 128], identity=ident[:]),
                              reads=["attnb", "ident"], writes=["ps6"])
                    dst = attnT[:].rearrange("p (f n) -> p f n", f=4)[:, :, i * 128:(i + 1) * 128]
                    src = psb[6][:, 512:1024].rearrange("p (f n) -> p f n", f=4)
                    S.add("dve", lambda e, dst=dst, src=src: e.tensor_copy(out=dst, in_=src), reads=["ps6"], writes=["attnT"])

                units = [(i, g) for i in range(NOWN) for g in range(2)]

                def late_S(u):
                    ui, ug = u
                    fin_late(ui, ug, 1, attn[ui % 2])
                    if ug == 1:
                        block_done(ui)

                for n, (i, g) in enumerate(units):
                    A = attn[i % 2]
                    sbk = selb[i % 2]
                    if g == 0:
                        S.add("sp", lambda e, i=i, sbk=sbk: e.dma_start(out=sbk[:], in_=selbias_d[i, :, :]), writes=[f"selb{i % 2}"], dma=True)
                    stage_C1(i, g, A, sbk)
                    stage_W(i, g)
                    if n >= 2:
                        late_S(units[n - 2])
                    if n >= 1:
                        pu = units[n - 1]
                        fin_late(pu[0], pu[1], 2, attn[pu[0] % 2])
                        stage_S(*pu)
                    stage_C2(i, g)
                    if n >= 1:
                        fin_copy(4, 1)
                    fin_copy(5, 2)
                U = units[-1]
                stage_S(*U)
                late_S(units[-2])
                fin_late(U[0], U[1], 2, attn[U[0] % 2])
                fin_copy(4, 1)
                late_S(U)
                if "p2b" in dbg:
                    es.dump_tmp = stg
                    dump(S, es, "attnT", attnT, BF16)
                    dump(S, es, "rstda", rstd_a, F32)
                    dump(S, es, "sel", seldbg, BF16)
                S.drain()
                with nc.Block() as block:
                    base = S.emit(block, sems)
            q_scope.close()
            if "stop2b" in dbg:
                r_scope.close()
                return nc

        with ExitStack() as x_scope:
            def xsb(name, shape, dt):
                return x_scope.enter_context(nc.sbuf_tensor("x_" + name, list(shape), dt))
            x1 = xsb("x1", [128, NOWN * 1024], F32)
            x1T = xsb("x1T", [128, 8 * 2048], BF16)
            comb = xsb("comb", [128, NOWN * 16], F32)
            nhalf16 = xsb("nhalf16", [128, 16], F32)

            with ExitStack() as es:
                def sb(name, shape, dt):
                    return es.enter_context(nc.sbuf_tensor("p2c_" + name, list(shape), dt))
                S = Sched(base)
                wo = sb("wo", [128, 8 * 1024], BF16)
                wst = sb("wst", [128, 1024], F32)
                gain = sb("gain", [128, 8], F32)
                xres = [sb("xres0", [128, 1024], F32)] * 2
                t1 = [sb(f"t1{k}", [128, 1024], F32) for k in range(2)]
                lng = sb("lng", [128, 1024], F32)
                lnb = sb("lnb", [128, 1024], F32)
                stats = sb("stats", [128, 12], F32)
                mv = sb("mv", [128, 2], F32)
                rstd = sb("rstd", [128, 1], F32)
                x1b = [sb(f"x1b{k}", [128, 1024], BF16) for k in range(2)]
                rstd_r = sb("rstd_r", [128, NOWN], F32)
                S.add("pool", lambda e: e.memset(nhalf16[:], -0.5), writes=["nhalf16"])
                S.add("sp", lambda e: e.dma_start(out=gain[:], in_=gain_d[:, :]), writes=["gain"], dma=True)
                S.add("sp", lambda e: e.dma_start(out=lng[:], in_=lnp_d[0:1, :].partition_broadcast(128)), writes=["lng"], dma=True)
                S.add("sp", lambda e: e.dma_start(out=lnb[:], in_=lnp_d[1:2, :].partition_broadcast(128)), writes=["lnb"], dma=True)
                S.add("sp", lambda e: e.dma_start(out=xres[0][:], in_=xs[3 * 128:4 * 128, :]), writes=["xres0"], dma=True)
                S.add("act", lambda e: e.activation(out=xres[0][:], in_=xres[0][:], func=AF.Copy, scale=ALPHA), reads=["xres0"], writes=["xres0"])
                for dc in range(8):
                    wb = wst if dc % 2 == 0 else t1[1]
                    wk = "wst" if dc % 2 == 0 else "t11"
                    S.add("sp", lambda e, dc=dc, wb=wb: e.dma_start(out=wb[:], in_=wout_d[dc * 128:(dc + 1) * 128, :]), writes=[wk], dma=True)
                    S.add("dve", lambda e, dc=dc, wb=wb: e.tensor_scalar(out=wo[:, dc * 1024:(dc + 1) * 1024], in0=wb[:], scalar1=gain[:, dc:dc + 1], scalar2=None, op0=ALU.mult),
                          reads=[wk, "gain"], writes=[f"wo{dc}"])
                wo_keys = [f"wo{dc}" for dc in range(8)]
                S.add("dve", lambda e: e.tensor_scalar(out=rstd_r[:], in0=ssr[:], scalar1=1.0 / 512.0, scalar2=RMS_EPS, op0=ALU.mult, op1=ALU.add), reads=["ssr"], writes=["rstd_r"])
                S.add("act", lambda e: e.activation(out=rstd_r[:], in_=rstd_r[:], func=AF.Ln), reads=["rstd_r"], writes=["rstd_r"])
                S.add("act", lambda e: e.activation(out=rstd_r[:], in_=rstd_r[:], func=AF.Exp, scale=-0.5), reads=["rstd_r"], writes=["rstd_r"])

                def do_tile_2c(i):
                    k = i % 2
                    L = 4 * i + 3
                    if i > 0:
                        S.add("sp", lambda e: e.dma_start(out=xres[k][:], in_=xs[L * 128:(L + 1) * 128, :]), writes=["xres0"], dma=True)
                        S.add("act", lambda e: e.activation(out=xres[k][:], in_=xres[k][:], func=AF.Copy, scale=ALPHA), reads=["xres0"], writes=["xres0"])
                    for src, sk, b0 in ((attnT, "attnT", 0), (rnnT, "rnnT", 2)):
                        for half in range(2):
                            for fc in range(4):
                                wrow = (fc if b0 == 0 else 4 + fc)
                                S.add("pe", lambda e, src=src, half=half, fc=fc, wrow=wrow, b0=b0: e.matmul(
                                    ps[b0 + half][:, :], lhsT=src[:, fc * 2048 + i * 128: fc * 2048 + (i + 1) * 128],
                                    rhs=wo[:, wrow * 1024 + half * 512: wrow * 1024 + (half + 1) * 512], start=(fc == 0), stop=(fc == 3)),
                                    reads=[sk, f"wo{wrow}"], writes=[f"ps{b0 + half}"])
                    T = t1[k]
                    for half in range(2):
                        hs = slice(half * 512, (half + 1) * 512)
                        S.add("dve", lambda e, half=half, hs=hs: e.scalar_tensor_tensor(out=T[:, hs], in0=ps[half][:, :], scalar=rstd_a[:, i:i + 1], in1=xres[k][:, hs],
                                                                                    op0=ALU.mult, op1=ALU.add),
                              reads=[f"ps{half}", "rstd_a", "xres0"], writes=[f"t1{k}"])
                        S.add("dve", lambda e, half=half, hs=hs: e.scalar_tensor_tensor(out=T[:, hs], in0=ps[2 + half][:, :], scalar=rstd_r[:, i:i + 1], in1=T[:, hs],
                                                                                    op0=ALU.mult, op1=ALU.add),
                              reads=[f"ps{2 + half}", "rstd_r", f"t1{k}"], writes=[f"t1{k}"])
                    for half in range(2):
                        S.add("dve", lambda e, half=half: e.bn_stats(out=stats[:, half * 6:(half + 1) * 6], in_=T[:, half * 512:(half + 1) * 512]),
                              reads=[f"t1{k}", "stats"], writes=["stats"])
                    S.add("dve", lambda e: e.bn_aggr(out=mv[:], in_=stats[:]), reads=["stats"], writes=["mv"])
                    S.add("dve", lambda e: e.tensor_scalar(out=rstd[:], in0=mv[:, 1:2], scalar1=LN_EPS, scalar2=None, op0=ALU.add), reads=["mv"], writes=["rstd"])
                    S.add("act", lambda e: e.activation(out=rstd[:], in_=rstd[:], func=AF.Ln), reads=["rstd"], writes=["rstd"])
                    S.add("act", lambda e: e.activation(out=rstd[:], in_=rstd[:], func=AF.Exp, scale=-0.5), reads=["rstd"], writes=["rstd"])
                    S.add("dve", lambda e: e.scalar_tensor_tensor(out=T[:], in0=T[:], scalar=mv[:, 0:1], in1=lng[:], op0=ALU.subtract, op1=ALU.mult),
                          reads=[f"t1{k}", "mv", "lng"], writes=[f"t1{k}"])
                    X1 = x1[:, i * 1024:(i + 1) * 1024]
                    S.add("dve", lambda e: e.scalar_tensor_tensor(out=X1, in0=T[:], scalar=rstd[:, 0:1], in1=lnb[:], op0=ALU.mult, op1=ALU.add),
                          reads=[f"t1{k}", "rstd", "lnb"], writes=[f"x1_{i}"])

                def do_tile_2c_B(i):
                    k = i % 2
                    X1 = x1[:, i * 1024:(i + 1) * 1024]
                    S.add("act", lambda e: e.copy(out=x1b[k][:], in_=X1), reads=[f"x1_{i}"], writes=[f"x1b{k}"])
                    pb = psb[4]
                    for dc in range(8):
                        S.add("pe", lambda e, dc=dc: e.transpose(out=pb[:, dc * 128:(dc + 1) * 128], in_=x1b[k][:, dc * 128:(dc + 1) * 128], identity=ident[:]),
                              reads=[f"x1b{k}", "ident"], writes=["ps4"])
                    dst = x1T[:].rearrange("p (c n) -> p c n", c=8)[:, :, i * 128:(i + 1) * 128]
                    srcp = pb[:].rearrange("p (c n) -> p c n", c=8)
                    S.add("act", lambda e: e.copy(out=dst, in_=srcp), reads=["ps4"], writes=["x1T"])

                wg = sb("wg", [128, 8 * 1024], BF16)
                wp = sb("wp", [128, 2 * 1024], BF16)
                wr = sb("wr", [128, 8 * 20], BF16)
                wrs = sb("wrs", [128, 8 * 20], F32)
                wst3 = wst
                rbias = sb("rbias", [128, 20], F32)
                pst = [sb(f"pst{k}", [128, 256], F32) for k in range(2)]
                pbb = [sb(f"pbb{k}", [128, 256], BF16) for k in range(2)]
                pT = [sb(f"pT{k}", [128, 256], BF16) for k in range(2)]
                sg = [sb("sg0", [128, 1024], F32)] * 2
                lg = sb("lg", [128, 20], F32)
                sm = sb("sm", [128, 16], F32)
                eg = sb("eg", [128, 4], F32)
                ohg = sb("ohg", [128, 4], F32)
                lsel = sb("lsel", [128, 8], F32)
                m8 = sb("m8", [128, 8], F32)
                wsel = sb("wsel", [128, 4], F32)
                wsel2 = sb("wsel2", [128, 4], F32)
                wg_keys = [f"wg{dc}" for dc in range(8)]

                def do_tile_3a(i):
                    k = i % 2
                    for dc in range(8):
                        S.add("pe", lambda e, dc=dc: e.matmul(ps[5][:, 0:20], lhsT=x1T[:, dc * 2048 + i * 128: dc * 2048 + (i + 1) * 128], rhs=wr[:, dc * 20:(dc + 1) * 20],
                                                               start=(dc == 0), stop=(dc == 7)),
                              reads=["x1T", "wr"], writes=["ps5"])
                    S.add("dve", lambda e: e.tensor_tensor(out=lg[:], in0=ps[5][:, 0:20], in1=rbias[:], op=ALU.add), reads=["ps5", "rbias"], writes=["lg"])
                    S.add("dve", lambda e: e.tensor_reduce(out=sm[:, 0:1], in_=lg[:, 0:4], axis=mybir.AxisListType.X, op=ALU.max), reads=["lg", "sm"], writes=["sm"])
                    S.add("dve", lambda e: e.tensor_scalar(out=sm[:, 1:2], in0=sm[:, 0:1], scalar1=-1.0, scalar2=None, op0=ALU.mult), reads=["sm"], writes=["sm"])
                    S.add("act", lambda e: e.activation(out=eg[:], in_=lg[:, 0:4], func=AF.Exp, bias=sm[:, 1:2], scale=1.0, accum_out=sm[:, 2:3]), reads=["lg", "sm"], writes=["eg", "sm"])
                    S.add("dve", lambda e: e.tensor_scalar(out=ohg[:], in0=lg[:, 0:4], scalar1=sm[:, 0:1], scalar2=None, op0=ALU.is_equal), reads=["lg", "sm"], writes=["ohg"])
                    S.add("dve", lambda e: e.tensor_scalar(out=lsel[:, 0:4], in0=lg[:, 4:8], scalar1=ohg[:, 0:1], scalar2=None, op0=ALU.mult), reads=["lg", "ohg", "lsel"], writes=["lsel"])
                    for g in range(1, 4):
                        S.add("dve", lambda e, g=g: e.scalar_tensor_tensor(out=lsel[:, 0:4], in0=lg[:, 4 + 4 * g:8 + 4 * g], scalar=ohg[:, g:g + 1], in1=lsel[:, 0:4],
                                                                          op0=ALU.mult, op1=ALU.add),
                              reads=["lg", "ohg", "lsel"], writes=["lsel"])
                    S.add("dve", lambda e: e.max(out=m8[:], in_=lsel[:]), reads=["lsel"], writes=["m8"])
                    S.add("dve", lambda e: e.tensor_tensor(out=sm[:, 4:5], in0=m8[:, 1:2], in1=m8[:, 0:1], op=ALU.subtract), reads=["m8", "sm"], writes=["sm"])
                    S.add("act", lambda e: e.activation(out=sm[:, 5:6], in_=sm[:, 4:5], func=AF.Exp), reads=["sm"], writes=["sm"])
                    S.add("dve", lambda e: e.tensor_scalar(out=sm[:, 6:7], in0=sm[:, 5:6], scalar1=1.0, scalar2=sm[:, 2:3], op0=ALU.add, op1=ALU.mult), reads=["sm"], writes=["sm"])
                    S.add("dve", lambda e: e.reciprocal(out=sm[:, 7:8], in_=sm[:, 6:7]), reads=["sm"], writes=["sm"])
                    S.add("dve", lambda e: e.tensor_tensor(out=sm[:, 8:9], in0=sm[:, 7:8], in1=sm[:, 5:6], op=ALU.mult), reads=["sm"], writes=["sm"])
                    S.add("dve", lambda e: e.tensor_scalar(out=wsel[:], in0=lsel[:, 0:4], scalar1=m8[:, 0:1], scalar2=sm[:, 7:8], op0=ALU.is_equal, op1=ALU.mult),
                          reads=["lsel", "m8", "sm"], writes=["wsel"])
                    S.add("dve", lambda e: e.tensor_scalar(out=wsel2[:], in0=lsel[:, 0:4], scalar1=m8[:, 1:2], scalar2=sm[:, 8:9], op0=ALU.is_equal, op1=ALU.mult),
                          reads=["lsel", "m8", "sm"], writes=["wsel2"])
                    S.add("dve", lambda e: e.tensor_tensor(out=wsel[:], in0=wsel[:], in1=wsel2[:], op=ALU.add), reads=["wsel", "wsel2"], writes=["wsel"])
                    cdst = comb[:, i * 16:(i + 1) * 16].rearrange("p (g e) -> p g e", g=4)
                    S.add("dve", lambda e: e.tensor_tensor(out=cdst, in0=ohg[:, 0:4].unsqueeze(2).to_broadcast([128, 4, 4]),
                                                           in1=wsel[:, 0:4].unsqueeze(1).to_broadcast([128, 4, 4]), op=ALU.mult),
                          reads=["wsel", "ohg"], writes=["comb"])
                    S.add("sp", lambda e: e.dma_start(out=pst[k][:], in_=po[i * 128:(i + 1) * 128, :]), writes=[f"pst{k}"], dma=True)
                    S.add("pool", lambda e: e.tensor_copy(out=pbb[k][:], in_=pst[k][:]), reads=[f"pst{k}"], writes=[f"pbb{k}"])
                    for kc in range(2):
                        S.add("pe", lambda e, kc=kc: e.transpose(out=psb[5][:, 512 + kc * 128:512 + (kc + 1) * 128], in_=pbb[k][:, kc * 128:(kc + 1) * 128], identity=ident[:]),
                              reads=[f"pbb{k}", "ident"], writes=["ps5"])
                    S.add("dve", lambda e: e.tensor_copy(out=pT[k][:], in_=psb[5][:, 512:768]), reads=["ps5"], writes=[f"pT{k}"])
                    G_ = sg[k]
                    for half in range(2):
                        for dc in range(8):
                            S.add("pe", lambda e, half=half, dc=dc: e.matmul(ps[6 + half][:, :], lhsT=x1T[:, dc * 2048 + i * 128: dc * 2048 + (i + 1) * 128],
                                                                           rhs=wg[:, dc * 1024 + half * 512: dc * 1024 + (half + 1) * 512], start=(dc == 0), stop=(dc == 7)),
                                  reads=["x1T", f"wg{dc}"], writes=[f"ps{6 + half}"])
                    for half in range(2):
                        hs = slice(half * 512, (half + 1) * 512)
                        S.add("act", lambda e, half=half, hs=hs: e.activation(out=G_[:, hs], in_=ps[6 + half][:, :], func=AF.Tanh, scale=0.5), reads=[f"ps{6 + half}"], writes=["sg0"])
                    for half in range(2):
                        for kc in range(2):
                            S.add("pe", lambda e, half=half, kc=kc: e.matmul(ps[6 + half][:, :], lhsT=pT[k][:, kc * 128:(kc + 1) * 128],
                                                                           rhs=wp[:, kc * 1024 + half * 512: kc * 1024 + (half + 1) * 512], start=(kc == 0), stop=(kc == 1)),
                                  reads=[f"pT{k}", f"wp{kc}"], writes=[f"ps{6 + half}"])
                    for half in range(2):
                        hs = slice(half * 512, (half + 1) * 512)
                        S.add("dve", lambda e, half=half, hs=hs: e.scalar_tensor_tensor(out=G_[:, hs], in0=G_[:, hs], scalar=1.0, in1=ps[6 + half][:, :], op0=ALU.add, op1=ALU.mult),
                              reads=["sg0", f"ps{6 + half}"], writes=["sg0"])
                    X1 = x1[:, i * 1024:(i + 1) * 1024]
                    S.add("dve", lambda e: e.scalar_tensor_tensor(out=X1, in0=X1, scalar=ALPHA, in1=G_[:], op0=ALU.mult, op1=ALU.add), reads=["sg0", f"x1_{i}"], writes=[f"x1_{i}"])

                do_tile_2c(0)
                S.add("pool", lambda e: e.memset(lsel[:], -1.0e30), writes=["lsel"])
                S.add("sp", lambda e: e.dma_start(out=rbias[:], in_=rb_d[0:1, :].partition_broadcast(128)), writes=["rbias"], dma=True)
                S.add("sp", lambda e: e.dma_start(out=wrs[:].rearrange("p (c n) -> p c n", c=8), in_=wr_d[:, :].rearrange("(c p) n -> p c n", p=128)), writes=["wrs"], dma=True)
                S.add("dve", lambda e: e.tensor_copy(out=wr[:], in_=wrs[:]), reads=["wrs"], writes=["wr"])
                for dc in range(8):
                    S.add("sp", lambda e, dc=dc: e.dma_start(out=wst3[:], in_=wg_d[dc * 128:(dc + 1) * 128, :]), writes=["wst"], dma=True)
                    S.add("dve", lambda e, dc=dc: e.tensor_copy(out=wg[:, dc * 1024:(dc + 1) * 1024], in_=wst3[:]), reads=["wst"], writes=[f"wg{dc}"])
                for kc in range(2):
                    S.add("sp", lambda e, kc=kc: e.dma_start(out=wst3[:], in_=wp_d[kc * 128:(kc + 1) * 128, :]), writes=["wst"], dma=True)
                    S.add("dve", lambda e, kc=kc: e.tensor_scalar(out=wp[:, kc * 1024:(kc + 1) * 1024], in0=wst3[:], scalar1=0.5, scalar2=None, op0=ALU.mult), reads=["wst"], writes=[f"wp{kc}"])
                for i in range(NOWN):
                    if i + 1 < NOWN:
                        do_tile_2c(i + 1)
                    if i >= 1:
                        do_tile_3a(i - 1)
                    do_tile_2c_B(i)
                do_tile_3a(NOWN - 1)
                if "p2c" in dbg:
                    es.dump_tmp = wst
                    dump(S, es, "x1", x1, F32)
                S.drain()
                with nc.Block() as block:
                    base = S.emit(block, sems)
            r_scope.close()
            if "stop2c" in dbg:
                return nc

            with ExitStack() as es:
                def sb(name, shape, dt):
                    return es.enter_context(nc.sbuf_tensor("p3b_" + name, list(shape), dt))
                S = Sched(base)
                wu = [sb(f"wu{k}", [128, 8 * 1024], BF16) for k in range(2)]
                wd = [sb(f"wd{k}", [128, 4 * 1024], BF16) for k in range(2)]
                NST = 3
                wst = [sb(f"wst{k}", [128, 1024], F32) for k in range(NST)]
                sa = [sb(f"sa{k}", [128, 512], F32) for k in range(2)]
                H = [[sb(f"H{p}_{fc}", [128, 512], BF16) for fc in range(4)] for p in range(2)]
                lng = sb("lng", [128, 1024], F32)
                lnb = sb("lnb", [128, 1024], F32)
                stats = sb("stats", [128, 12], F32)
                mv = sb("mv", [128, 2], F32)
                rstd = sb("rstd", [128, 1], F32)
                yb = [sb(f"yb{k}", [128, 1024], F32) for k in range(2)]
                S.add("sp", lambda e: e.dma_start(out=lng[:], in_=lnp_d[2:3, :].partition_broadcast(128)), writes=["lng"], dma=True)
                S.add("sp", lambda e: e.dma_start(out=lnb[:], in_=lnp_d[3:4, :].partition_broadcast(128)), writes=["lnb"], dma=True)
                stc = [0]

                def load_expert(e_):
                    p = e_ % 2
                    for kind, nchunk, srcd, dstt in (("u", 8, wup_d, wu[p]), ("d", 4, wdn_d, wd[p])):
                        for c in range(nchunk):
                            sidx = stc[0] % NST
                            stc[0] += 1
                            st = wst[sidx]
                            S.add("sp", lambda e, srcd=srcd, c=c, st=st: e.dma_start(out=st[:], in_=srcd[e_, c * 128:(c + 1) * 128, :]), writes=[f"wst{sidx}"], dma=True)
                            key = f"w{kind}{p}_{c}"
                            ceng = ("pool", "dve", "act")[stc[0] % 3] if e_ == 0 else "pool"
                            if ceng == "act":
                                S.add("act", lambda e, dstt=dstt, c=c, st=st: e.copy(out=dstt[:, c * 1024:(c + 1) * 1024], in_=st[:]), reads=[f"wst{sidx}"], writes=[key])
                            else:
                                S.add(ceng, lambda e, dstt=dstt, c=c, st=st: e.tensor_copy(out=dstt[:, c * 1024:(c + 1) * 1024], in_=st[:]), reads=[f"wst{sidx}"], writes=[key])

                actr = [0]

                def up_chunk(e_, tc):
                    p = e_ % 2
                    hp = (e_ * 4 + tc) % 2
                    wkeys = [f"wu{p}_{c}" for c in range(8)]
                    for fc in range(4):
                        pa = (fc % 2) * 2
                        for ab in range(2):
                            m = fc + 4 * ab
                            for dc in range(8):
                                S.add("pe", lambda e, dc=dc, m=m, pa=pa, ab=ab: e.matmul(ps[pa + ab][:, :], lhsT=wu[p][:, dc * 1024 + m * 128: dc * 1024 + (m + 1) * 128],
                                                                                     rhs=x1T[:, dc * 2048 + tc * 512: dc * 2048 + (tc + 1) * 512], start=(dc == 0), stop=(dc == 7)),
                                      reads=["x1T", f"wu{p}_{dc}"], writes=[f"ps{pa + ab}"])
                        sk = actr[0] % 2
                        actr[0] += 1
                        S.add("act", lambda e, pa=pa, sk=sk: e.activation(out=sa[sk][:], in_=ps[pa][:, :], func=AF.Silu), reads=[f"ps{pa}"], writes=[f"sa{sk}"])
                        S.add("dve", lambda e, pa=pa, sk=sk, fc=fc: e.tensor_tensor(out=H[hp][fc][:], in0=sa[sk][:], in1=ps[pa + 1][:, :], op=ALU.mult),
                              reads=[f"sa{sk}", f"ps{pa + 1}"], writes=[f"H{hp}_{fc}"])

                dctr = [0]

                def down_chunk(e_, tc):
                    p = e_ % 2
                    hp = (e_ * 4 + tc) % 2
                    wkeys = [f"wd{p}_{c}" for c in range(4)]
                    for tt in range(4):
                        i = tc * 4 + tt
                        for half in range(2):
                            bank = 4 + dctr[0] % 4
                            dctr[0] += 1
                            for fc in range(4):
                                S.add("pe", lambda e, fc=fc, tt=tt, half=half, bank=bank: e.matmul(ps[bank][:, :], lhsT=H[hp][fc][:, tt * 128:(tt + 1) * 128],
                                                                                               rhs=wd[p][:, fc * 1024 + half * 512: fc * 1024 + (half + 1) * 512],
                                                                                               start=(fc == 0), stop=(fc == 3)),
                                      reads=[f"H{hp}_{fc}", f"wd{p}_{fc}"], writes=[f"ps{bank}"])
                            A_ = x1[:, i * 1024 + half * 512: i * 1024 + (half + 1) * 512]
                            S.add("dve", lambda e, bank=bank, A_=A_, i=i: e.scalar_tensor_tensor(out=A_, in0=ps[bank][:, :], scalar=comb[:, i * 16 + e_: i * 16 + e_ + 1], in1=A_,
                                                                                               op0=ALU.mult, op1=ALU.add),
                                  reads=[f"ps{bank}", "comb", f"x1_{i}"], writes=[f"x1_{i}"])
                        if e_ == 15:
                            ln2_store(i)

                def ln2_store(i):
                    k = i % 2
                    T = x1[:, i * 1024:(i + 1) * 1024]
                    Y = yb[k]
                    for half in range(2):
                        S.add("dve", lambda e, half=half: e.bn_stats(out=stats[:, half * 6:(half + 1) * 6], in_=T[:, half * 512:(half + 1) * 512]),
                              reads=[f"x1_{i}", "stats"], writes=["stats"])
                    S.add("dve", lambda e: e.bn_aggr(out=mv[:], in_=stats[:]), reads=["stats"], writes=["mv"])
                    S.add("dve", lambda e: e.tensor_scalar(out=rstd[:], in0=mv[:, 1:2], scalar1=LN_EPS, scalar2=None, op0=ALU.add), reads=["mv"], writes=["rstd"])
                    S.add("act", lambda e: e.activation(out=rstd[:], in_=rstd[:], func=AF.Ln), reads=["rstd"], writes=["rstd"])
                    S.add("act", lambda e: e.activation(out=rstd[:], in_=rstd[:], func=AF.Exp, scale=-0.5), reads=["rstd"], writes=["rstd"])
                    S.add("dve", lambda e: e.scalar_tensor_tensor(out=Y[:], in0=T, scalar=mv[:, 0:1], in1=lng[:], op0=ALU.subtract, op1=ALU.mult),
                          reads=[f"x1_{i}", "mv", "lng"], writes=[f"yb{k}"])
                    S.add("dve", lambda e: e.scalar_tensor_tensor(out=Y[:], in0=Y[:], scalar=rstd[:, 0:1], in1=lnb[:], op0=ALU.mult, op1=ALU.add),
                          reads=[f"yb{k}", "rstd", "lnb"], writes=[f"yb{k}"])
                    S.add("sp", lambda e: e.dma_start(out=out_d[i * 128:(i + 1) * 128, :], in_=Y[:]), reads=[f"yb{k}"], writes=[f"out{i}"], dma=True)

                load_expert(0)
                seq = [(e_, tc) for e_ in range(16) for tc in range(4)]
                prev = None
                for (e_, tc) in seq:
                    up_chunk(e_, tc)
                    if prev is not None:
                        down_chunk(*prev)
                    if tc == 0 and e_ + 1 < 16:
                        load_expert(e_ + 1)
                    prev = (e_, tc)
                down_chunk(*prev)
                S.drain()
                with nc.Block() as block:
                    base = S.emit(block, sems)
    return nc


def host_prep(inputs):
    f = lambda k: np.asarray(inputs[k], dtype=np.float32)
    x = f("x")
    p = f("p")[0]
    w_in = f("w_in")[0]
    rel_bias = f("rel_bias")
    q_cols = np.arange(0, 512)
    kv0 = 512
    kc, vc, ks, vs, kw, vw = [np.arange(kv0 + 128 * k, kv0 + 128 * (k + 1)) for k in range(6)]
    g_cols = np.arange(1280, 1304)
    rx = np.arange(1304, 1816)
    ry = np.arange(1816, 2328)
    colsA = np.concatenate([kc, vc, ks, kw, rx, vs, vw])
    qperm = np.concatenate([np.concatenate([np.arange(64 * r, 64 * r + 64), np.arange(64 * (4 + r), 64 * (4 + r) + 64)]) for r in range(4)])
    colsC = np.concatenate([q_cols[qperm], ry, g_cols])
    rep = {}
    rep["wA"] = np.ascontiguousarray(w_in[:, colsA])
    rep["wC"] = np.ascontiguousarray(w_in[:, colsC])
    ki = np.arange(128)[:, None]
    qi = np.arange(128)[None, :]
    tabs = np.zeros((NTAB, 128, 8, 128), np.float32)

    def fill(tid, d, valid):
        bk = _bucket_exact(d)
        for h in range(8):
            v = rel_bias[bk, h]
            tabs[tid, :, h, :] = np.where(valid, v, np.float32(MASKV))
    fill(T_D, qi - ki, (qi - ki) >= 0)
    fill(T_O1, 128 + qi - ki, np.ones((128, 128), bool))
    fill(T_W4, 512 + qi - ki, (512 + qi - ki) < 512)
    fill(T_C, np.full((128, 128), 1000), np.ones((128, 128), bool))
    for m in range(4):
        d = 512 * m + 369 + qi - 16 * ki
        fill(T_N0 + m, d, d >= 0)
    rep["tabs"] = tabs.reshape(NTAB, 128, 1024)
    rep["cb"] = np.ascontiguousarray(np.broadcast_to(rel_bias[31, :][None, :], (128, 8))).astype(np.float32)
    s_ix = np.arange(128)[:, None]
    c_ix = np.arange(SEQ)[None, :]
    rep["E"] = (c_ix // 64 == s_ix).astype(np.float32)
    conv_w = f("conv_w")[0]
    rep["convw"] = np.ascontiguousarray(conv_w.reshape(4, 4, 128).transpose(2, 0, 1).reshape(128, 16))
    vec = lambda k: f(k)[0].reshape(4, 128).T
    rep["rgvec"] = np.ascontiguousarray(np.concatenate([vec("conv_b"), vec("rg_b_a"), vec("rg_b_x"), vec("rg_lambda")], axis=1))

    def bd(w):
        o = np.zeros((128, 4, 128), np.float32)
        for ct in range(4):
            for e in range(2):
                o[e * 64:(e + 1) * 64, ct, e * 64:(e + 1) * 64] = w[2 * ct + e]
        return o.reshape(128, 512)
    rep["BDa"] = bd(f("rg_w_a")[0])
    rep["BDx"] = bd(f("rg_w_x")[0])

    def bdw(w):
        o = np.zeros((128, 32, 128), np.float32)
        wl = w.reshape(32, 64, 64)
        for g in range(2):
            o[g * 64:(g + 1) * 64, :, g * 64:(g + 1) * 64] = wl.transpose(1, 0, 2)
        return o.reshape(128, 32 * 128)
    rep["BDWk"] = bdw(f("cmp_w_k")[0])
    rep["BDWv"] = bdw(f("cmp_w_v")[0])
    pk = f("cmp_pe_k")[0].T
    pv = f("cmp_pe_v")[0].T
    rep["pe2"] = np.ascontiguousarray(np.concatenate([np.concatenate([pk, pk], 0), np.concatenate([pv, pv], 0)], axis=1))
    cp = np.arange(512)[:, None] - 1
    sl = np.arange(128)[None, :]
    ovm = ((16 * cp < 64 * sl + 64) & (16 * cp + 32 > 64 * sl)).astype(np.float32)
    rep["ov"] = np.ascontiguousarray(ovm.reshape(4, 128, 128).transpose(1, 0, 2).reshape(128, 512))
    gain = np.concatenate([f("attn_out_gain")[0], f("rnn_out_gain")[0]])
    rep["gain"] = np.ascontiguousarray(gain.reshape(8, 128).T)
    rep["wout"] = f("w_out")[0]
    rep["lnp"] = np.stack([f("ln1_g")[0], f("ln1_b")[0], f("ln2_g")[0], f("ln2_b")[0]])
    rep["wr"] = np.ascontiguousarray(np.concatenate([f("router_group_w")[0], f("router_expert_w")[0]], axis=1))
    rep["rb"] = np.concatenate([f("router_group_b")[0], f("router_expert_b")[0]])[None, :]
    rep["wup"] = f("expert_w_up")[0]
    rep["wdn"] = f("expert_w_down")[0]
    rep["wp"] = f("ple_w")[0]
    rep["wg"] = f("ple_gate_w")[0]
    in_maps = []
    for c in range(8):
        b, j = divmod(c, 4)
        sh = (3 - j) * 128
        m = dict(rep)
        xs_ = np.zeros((SEQ, D_MODEL), np.float32)
        xs_[sh:] = x[b, :SEQ - sh]
        m["xs"] = xs_
        own = np.concatenate([np.arange((4 * i + j) * 128, (4 * i + j + 1) * 128) for i in range(NOWN)])
        m["po"] = np.ascontiguousarray(p[b, own])
        kb = np.zeros((128, NT), np.float32)
        kb[:, :3 - j] = MASKV
        m["keybias"] = kb
        cpi = np.arange(512) - 1
        cbv = np.where(cpi >= 8 * (3 - j), 0.0, MASKV).astype(np.float32)
        m["cbias"] = np.ascontiguousarray(cbv.reshape(4, 128).T)
        sbias = np.zeros((NOWN, 128, 128), np.float32)
        s0 = 2 * (3 - j)
        for i in range(NOWN):
            qpos = 128 * (4 * i + 3) + np.arange(128)[:, None]
            cur = qpos // 64
            sblk = np.arange(128)[None, :]
            valid = (sblk <= cur) & (sblk >= s0)
            forced = (sblk == s0) | (sblk == cur) | (sblk == cur - 1)
            sbias[i] = np.where(valid, np.where(forced, 1e4, 0.0), -1e30)
        m["selbias"] = sbias
        pm = np.ones((128, 512), np.float32)
        pm[:, :sh] = 0.0
        m["padmask"] = pm
        in_maps.append(m)
    return in_maps


def kernel(**inputs):
    in_maps = host_prep(inputs)
    nc = build_program()
    res = run_bass_kernel_spmd(nc, in_maps, core_ids=list(range(8)))
    out = np.zeros((2, SEQ, D_MODEL), np.float32)
    for c in range(8):
        b, j = divmod(c, 4)
        o = res.results[c]["out"]
        for i in range(NOWN):
            n = 4 * i + j
            out[b, n * 128:(n + 1) * 128] = o[i * 128:(i + 1) * 128]
    return out
```
